# Optimizing a Trainium2 kernel written in Bass

```python
import functools
import numpy as np
import jax, jax.numpy as jnp
from jax import lax

D_MODEL = 2048
BATCH = 8
SEQ = 4096
DEPTH = 2

CTX_LEN = 256
GRID_W = 64
CHUNK = 64
BRANCH_W = D_MODEL // 2
N_BRANCH = 3
MLSTM_HEADS = 4
MLSTM_DH = BRANCH_W // MLSTM_HEADS
HGRN_HEADS = 8
HGRN_DH = BRANCH_W // HGRN_HEADS
RET_HEADS = 4
RET_DV = BRANCH_W // RET_HEADS
RET_DK = RET_DV // 2
RET_QK_W = RET_HEADS * RET_DK
CONV_K = 3
FFN_DIM = ((8 * D_MODEL // 3 + 255) // 256) * 256
N_EXPERTS = 8
TOP_K = 2
EXPERT_DIM = 7 * D_MODEL // 2
MOE_BLOCK = 256
N_DENSE = (DEPTH + 1) // 2
N_MOE = DEPTH // 2
ALPHA = (2 * DEPTH) ** 0.25
BETA = (8 * DEPTH) ** -0.25
EPS = 1e-6
NEG = -1e30
TINY = 1e-30
IN_SIZES = (2 * BRANCH_W, BRANCH_W, BRANCH_W, 4 * MLSTM_HEADS,
            BRANCH_W, BRANCH_W, BRANCH_W, BRANCH_W, BRANCH_W,
            RET_QK_W, RET_QK_W, BRANCH_W, BRANCH_W,
            N_BRANCH * D_MODEL)
N_IN = sum(IN_SIZES)

kernel_name = 'hybrid_bidir_mlstm_hgrn2_retention_moe'

f32 = jnp.float32


def layer_norm(x):
    xf = x.astype(f32)
    mu = xf.mean(-1, keepdims=True)
    var = jnp.mean(jnp.square(xf - mu), -1, keepdims=True)
    return ((xf - mu) * lax.rsqrt(var + EPS)).astype(x.dtype)


def modulate(x, shift, scale):
    return layer_norm(x) * (1 + scale) + shift


def post_norm(z, g, b):
    return layer_norm(z) * g + b


def heads(a, n):
    b, t, w = a.shape
    return a.reshape(b, t, n, w // n).transpose(0, 2, 1, 3)


def merge_heads_normed(h, rms):
    hf = h.astype(f32)
    if rms:
        hf = hf * lax.rsqrt(jnp.mean(hf * hf, -1, keepdims=True) + EPS)
    else:
        hf = layer_norm(hf)
    b, n, t, d = h.shape
    return hf.transpose(0, 2, 1, 3).reshape(b, t, n * d).astype(h.dtype)


def to_chunks(a):
    b, n, t = a.shape[:3]
    return jnp.moveaxis(a.reshape(b, n, t // CHUNK, CHUNK, *a.shape[3:]), 2, 0)


def from_chunks(o):
    nc, b, n, c = o.shape[:4]
    return jnp.moveaxis(o, 0, 2).reshape(b, n, nc * c, *o.shape[4:])


def causal_mask():
    return jnp.tril(jnp.ones((CHUNK, CHUNK), bool))


def mlstm_scan(state, q, k, v, log_i, log_f):
    dtype = v.dtype
    causal = causal_mask()
    xs = tuple(to_chunks(a.astype(f32)) for a in (q, k, v, log_i, log_f))

    def step(carry, inp):
        C, n, m = carry
        qc, kc, vc, li, lf = inp
        b = jnp.cumsum(lf, axis=-1)
        a_inter = b + m[..., None]
        d_intra = jnp.where(causal, b[..., :, None] - b[..., None, :] + li[..., None, :], NEG)
        m_t = jnp.maximum(a_inter, d_intra.max(-1))
        w_inter = jnp.exp(a_inter - m_t)
        s = jnp.einsum('bhtk,bhsk->bhts', qc, kc) * jnp.exp(d_intra - m_t[..., None])
        num = w_inter[..., None] * jnp.einsum('bhtk,bhkv->bhtv', qc, C) + jnp.einsum('bhts,bhsv->bhtv', s, vc)
        den = w_inter * jnp.einsum('bhtk,bhk->bht', qc, n) + s.sum(-1)
        h = num / jnp.maximum(jnp.abs(den), jnp.exp(-m_t))[..., None]
        g = b[..., -1:] - b + li
        m_new = jnp.maximum(b[..., -1] + m, g.max(-1))
        decay = jnp.exp(b[..., -1] + m - m_new)
        w_state = jnp.exp(g - m_new[..., None])
        C_new = decay[..., None, None] * C + jnp.einsum('bhs,bhsk,bhsv->bhkv', w_state, kc, vc)
        n_new = decay[..., None] * n + jnp.einsum('bhs,bhsk->bhk', w_state, kc)
        return (C_new, n_new, m_new), h

    state, hs = lax.scan(step, state, xs)
    return from_chunks(hs).astype(dtype), state


def gla_scan(state, q, k, v, log_f):
    dtype = v.dtype
    causal = causal_mask()
    xs = tuple(to_chunks(a.astype(f32)) for a in (q, k, v, log_f))

    def step(S, inp):
        qc, kc, vc, lf = inp
        b = jnp.cumsum(lf, axis=-2)
        rel = jnp.where(causal[:, :, None], b[:, :, :, None, :] - b[:, :, None, :, :], NEG)
        a = jnp.einsum('bhtk,bhsk,bhtsk->bhts', qc, kc, jnp.exp(rel))
        o = jnp.einsum('bhtk,bhkv->bhtv', qc * jnp.exp(b), S) + jnp.einsum('bhts,bhsv->bhtv', a, vc)
        b_last = b[:, :, -1:, :]
        S = jnp.exp(b_last)[:, :, 0, :, None] * S + jnp.einsum('bhsk,bhsv->bhkv', kc * jnp.exp(b_last - b), vc)
        return S, o

    state, os_ = lax.scan(step, state, xs)
    return from_chunks(os_).astype(dtype), state


def retention_scan(state, q, k, v, log_gamma):
    dtype = v.dtype
    causal = causal_mask().astype(f32)
    pos = jnp.arange(1, CHUNK + 1, dtype=f32)
    lg = log_gamma.astype(f32)[:, None]
    inter_w = jnp.exp(lg * pos)[:, :, None]
    state_w = jnp.exp(lg * (CHUNK - pos))[:, :, None]
    chunk_decay = jnp.exp(lg * CHUNK)[:, :, None]
    rel = jnp.where(causal > 0, pos[:, None] - pos[None, :], 0.0)
    dmat = jnp.exp(lg[:, :, None] * rel) * causal
    xs = tuple(to_chunks(a.astype(f32)) for a in (q, k, v))

    def step(S, inp):
        qc, kc, vc = inp
        o = inter_w * jnp.einsum('bhtk,bhkv->bhtv', qc, S) + jnp.einsum(
            'bhts,bhsv->bhtv', jnp.einsum('bhtk,bhsk->bhts', qc, kc) * dmat, vc)
        S = chunk_decay * S + jnp.einsum('bhsk,bhsv->bhkv', kc * state_w, vc)
        return S, o

    state, os_ = lax.scan(step, state, xs)
    return from_chunks(os_).astype(dtype), state


def bidirectional(scan_fw, scan_bw, ctx_fw, ctx_bw, lat_fw, lat_bw, state0, with_ctx):
    flip = lambda args: tuple(jnp.flip(a, axis=2) for a in args)
    oc_fw, sc_fw = scan_fw(state0, *ctx_fw)
    oc_bw, sc_bw = scan_bw(state0, *flip(ctx_bw))
    ol_fw, _ = scan_fw(sc_fw, *lat_fw)
    ol_bw, _ = scan_bw(sc_bw, *flip(lat_bw))
    lat = ol_fw + jnp.flip(ol_bw, axis=2)
    ctx_out = oc_fw + jnp.flip(oc_bw, axis=2) if with_ctx else None
    return lat, ctx_out


def short_conv(a, w, b, grid):
    ch = a.shape[-1]
    if grid:
        bsz, t, _ = a.shape
        rows = t // GRID_W
        out = lax.conv_general_dilated(a.reshape(bsz, rows, GRID_W, ch), w[:, :, None, :], (1, 1), 'SAME',
                                       dimension_numbers=('NHWC', 'HWIO', 'NHWC'),
                                       feature_group_count=ch).reshape(bsz, t, ch)
    else:
        out = lax.conv_general_dilated(a, w[CONV_K // 2][:, None, :], (1,), 'SAME',
                                       dimension_numbers=('NWC', 'WIO', 'NWC'), feature_group_count=ch)
    return out + b


def mixer_inputs(u, w_in_l, conv_w_l, conv_b_l, mgate_b_l, lb_l, grid):
    bsz, t, _ = u.shape
    split_idx = np.cumsum(IN_SIZES)[:-1].tolist()
    (mqk, mv, mz, mgate, hq, hi, hf_fw, hf_bw, hg, rq, rk, rv, rg, merge) = jnp.split(u @ w_in_l, split_idx, axis=-1)
    mq, mk = jnp.split(jax.nn.silu(short_conv(mqk, conv_w_l, conv_b_l, grid)), 2, axis=-1)
    gates = (mgate + mgate_b_l).astype(f32).reshape(bsz, t, 4, MLSTM_HEADS).transpose(2, 0, 3, 1)
    mq_h = heads(mq, MLSTM_HEADS)
    mk_h = heads(mk, MLSTM_HEADS) * MLSTM_DH ** -0.5
    mv_h = heads(mv, MLSTM_HEADS)
    mlstm_fw = (mq_h, mk_h, mv_h, gates[0], jax.nn.log_sigmoid(gates[1]))
    mlstm_bw = (mq_h, mk_h, mv_h, gates[2], jax.nn.log_sigmoid(gates[3]))
    hq_h = heads(jax.nn.silu(hq), HGRN_HEADS)
    hv_h = heads(hi, HGRN_HEADS)
    hgrn = []
    for d, hf in enumerate((hf_fw, hf_bw)):
        lb = lb_l[d].reshape(1, HGRN_HEADS, 1, HGRN_DH)
        ft = heads(hf, HGRN_HEADS).astype(f32)
        one_minus_f = (1.0 - lb) * jax.nn.sigmoid(-ft)
        log_f = jnp.log(jnp.maximum(lb + (1.0 - lb) * jax.nn.sigmoid(ft), TINY))
        hgrn.append((hq_h, one_minus_f, hv_h, log_f))
    ret = (heads(rq, RET_HEADS), heads(rk, RET_HEADS) * RET_DK ** -0.5, heads(rv, RET_HEADS))
    return {'mlstm_fw': mlstm_fw, 'mlstm_bw': mlstm_bw, 'hgrn_fw': hgrn[0], 'hgrn_bw': hgrn[1],
            'ret': ret, 'post': (mz, hg, rg, merge)}


def merge_branches(ym, yh, yr, post, norm_g, w_br, w_o):
    mz, hg, rg, merge = post
    bsz, t, _ = mz.shape
    branches = (merge_heads_normed(ym, False) * norm_g[0] * jax.nn.silu(mz),
                merge_heads_normed(yh, True) * norm_g[1] * jax.nn.silu(hg),
                merge_heads_normed(yr, False) * norm_g[2] * jax.nn.silu(rg))
    gates = jax.nn.sigmoid(merge.reshape(bsz, t, N_BRANCH, D_MODEL))
    mixed = gates[:, :, 0] * (branches[0] @ w_br[0])
    for n in range(1, N_BRANCH):
        mixed = mixed + gates[:, :, n] * (branches[n] @ w_br[n])
    return mixed @ w_o


def swiglu(u, w_gate, w_up, w_down):
    return (jax.nn.silu(u @ w_gate) * (u @ w_up)) @ w_down


def moe_swiglu(u, w_router, w_gate, w_up, w_down):
    shp = u.shape
    xt = u.reshape(-1, D_MODEL)
    t = xt.shape[0]
    logits = (xt @ w_router).astype(f32)
    top_v, top_e = lax.top_k(logits, TOP_K)
    weights = jax.nn.softmax(top_v, axis=-1)
    e_flat = top_e.reshape(-1)
    tok_flat = jnp.repeat(jnp.arange(t), TOP_K)
    w_flat = weights.reshape(-1)
    n_assign = t * TOP_K
    order = jnp.argsort(e_flat)
    e_sorted, tok_sorted, w_sorted = e_flat[order], tok_flat[order], w_flat[order]
    sizes = jnp.bincount(e_flat, length=N_EXPERTS)
    padded = ((sizes + MOE_BLOCK - 1) // MOE_BLOCK) * MOE_BLOCK
    pad_end = jnp.cumsum(padded)
    pad_start = pad_end - padded
    grp_start = jnp.cumsum(sizes) - sizes
    dest = pad_start[e_sorted] + jnp.arange(n_assign) - grp_start[e_sorted]
    n_blocks = -(-n_assign // MOE_BLOCK) + N_EXPERTS
    buf = jnp.zeros((n_blocks * MOE_BLOCK, D_MODEL), u.dtype).at[dest].set(xt[tok_sorted])
    block_e = jnp.minimum(jnp.searchsorted(pad_end, jnp.arange(n_blocks) * MOE_BLOCK, side='right'), N_EXPERTS - 1)

    def expert_block(args):
        xb, e = args
        return swiglu(xb, w_gate[e], w_up[e], w_down[e])

    yb = lax.map(expert_block, (buf.reshape(n_blocks, MOE_BLOCK, D_MODEL), block_e))
    y = yb.reshape(-1, D_MODEL)[dest] * w_sorted[:, None].astype(u.dtype)
    out = jnp.zeros_like(xt).at[tok_sorted].add(y)
    return out.reshape(shp)


def setup_inputs(seed: int = 0) -> dict:
    key = jax.random.key(seed)
    ks = jax.random.split(key, 24)
    nrm = lambda k, shape, s: jax.random.normal(k, shape, f32) * s
    x = nrm(ks[0], (BATCH, SEQ, D_MODEL), 1.0)
    c = nrm(ks[1], (BATCH, D_MODEL), 1.0)
    ctx = nrm(ks[2], (BATCH, CTX_LEN, D_MODEL), 1.0)
    c_ctx = nrm(ks[3], (D_MODEL,), 1.0)
    w_ada = nrm(ks[4], (DEPTH, D_MODEL, 6 * D_MODEL), 0.5 * D_MODEL ** -0.5)
    b_ada = nrm(ks[5], (DEPTH, 6 * D_MODEL), 0.01)
    w_in = nrm(ks[6], (DEPTH, D_MODEL, N_IN), D_MODEL ** -0.5)
    conv_w = nrm(ks[7], (DEPTH, CONV_K, CONV_K, 2 * BRANCH_W), 1.0 / CONV_K)
    conv_b = nrm(ks[8], (DEPTH, 2 * BRANCH_W), 0.01)
    kig, kfg = jax.random.split(ks[9])
    ig = nrm(kig, (DEPTH, 2, MLSTM_HEADS), 0.1)
    fg = jnp.linspace(3.0, 6.0, MLSTM_HEADS) + nrm(kfg, (DEPTH, 2, MLSTM_HEADS), 0.1)
    mlstm_gate_b = jnp.stack([ig[:, 0], fg[:, 0], ig[:, 1], fg[:, 1]], axis=1).reshape(DEPTH, 4 * MLSTM_HEADS)
    hgrn_lb = nrm(ks[10], (2, DEPTH, BRANCH_W), 0.1)
    ret_decay_logit = jnp.log(2.0 ** (5.0 + jnp.arange(RET_HEADS, dtype=f32)) - 1.0) + nrm(ks[11], (DEPTH, 2, RET_HEADS), 0.1)
    head_norm_g = 1.0 + nrm(ks[12], (DEPTH, N_BRANCH, BRANCH_W), 0.02)
    w_branch = nrm(ks[13], (DEPTH, N_BRANCH, BRANCH_W, D_MODEL), BETA * BRANCH_W ** -0.5)
    w_out = nrm(ks[14], (DEPTH, D_MODEL, D_MODEL), BETA * D_MODEL ** -0.5)
    post_ln_g = 1.0 + nrm(ks[15], (DEPTH, 2, D_MODEL), 0.02)
    post_ln_b = nrm(ks[16], (DEPTH, 2, D_MODEL), 0.02)
    ffn_w_gate = nrm(ks[17], (N_DENSE, D_MODEL, FFN_DIM), D_MODEL ** -0.5)
    ffn_w_up = nrm(ks[18], (N_DENSE, D_MODEL, FFN_DIM), BETA * D_MODEL ** -0.5)
    ffn_w_down = nrm(ks[19], (N_DENSE, FFN_DIM, D_MODEL), BETA * FFN_DIM ** -0.5)
    moe_w_router = nrm(ks[20], (N_MOE, D_MODEL, N_EXPERTS), D_MODEL ** -0.5)
    moe_w_gate = nrm(ks[21], (N_MOE, N_EXPERTS, D_MODEL, EXPERT_DIM), D_MODEL ** -0.5)
    moe_w_up = nrm(ks[22], (N_MOE, N_EXPERTS, D_MODEL, EXPERT_DIM), BETA * D_MODEL ** -0.5)
    moe_w_down = nrm(ks[23], (N_MOE, N_EXPERTS, EXPERT_DIM, D_MODEL), BETA * EXPERT_DIM ** -0.5)
    return {'x': x, 'c': c, 'ctx': ctx, 'c_ctx': c_ctx, 'w_ada': w_ada, 'b_ada': b_ada,
            'w_in': w_in, 'conv_w': conv_w, 'conv_b': conv_b, 'mlstm_gate_b': mlstm_gate_b,
            'hgrn_lb': hgrn_lb, 'ret_decay_logit': ret_decay_logit, 'head_norm_g': head_norm_g,
            'w_branch': w_branch, 'w_out': w_out, 'post_ln_g': post_ln_g, 'post_ln_b': post_ln_b,
            'ffn_w_gate': ffn_w_gate, 'ffn_w_up': ffn_w_up, 'ffn_w_down': ffn_w_down,
            'moe_w_router': moe_w_router, 'moe_w_gate': moe_w_gate, 'moe_w_up': moe_w_up,
            'moe_w_down': moe_w_down}


def reference(x, c, ctx, c_ctx, w_ada, b_ada, w_in, conv_w, conv_b, mlstm_gate_b, hgrn_lb,
              ret_decay_logit, head_norm_g, w_branch, w_out, post_ln_g, post_ln_b,
              ffn_w_gate, ffn_w_up, ffn_w_down, moe_w_router, moe_w_gate, moe_w_up, moe_w_down):
    bsz = x.shape[0]
    lbs = jax.nn.softmax(hgrn_lb.astype(f32), axis=1)
    lbs = jnp.cumsum(lbs, axis=1) - lbs[:, :1]
    m_state0 = (jnp.zeros((bsz, MLSTM_HEADS, MLSTM_DH, MLSTM_DH), f32),
                jnp.zeros((bsz, MLSTM_HEADS, MLSTM_DH), f32),
                jnp.full((bsz, MLSTM_HEADS), NEG, f32))
    h_state0 = jnp.zeros((bsz, HGRN_HEADS, HGRN_DH, HGRN_DH), f32)
    r_state0 = jnp.zeros((bsz, RET_HEADS, RET_DK, RET_DV), f32)
    h = ctx
    for l in range(DEPTH):
        need_ctx = l < DEPTH - 1
        mod_x = jnp.split((jax.nn.silu(c) @ w_ada[l] + b_ada[l])[:, None, :], 6, axis=-1)
        mod_h = jnp.split(jax.nn.silu(c_ctx) @ w_ada[l] + b_ada[l], 6, axis=-1)
        ix = mixer_inputs(modulate(x, mod_x[0], mod_x[1]), w_in[l], conv_w[l], conv_b[l],
                          mlstm_gate_b[l], lbs[:, l], True)
        ih = mixer_inputs(modulate(h, mod_h[0], mod_h[1]), w_in[l], conv_w[l], conv_b[l],
                          mlstm_gate_b[l], lbs[:, l], False)
        ym_x, ym_h = bidirectional(mlstm_scan, mlstm_scan, ih['mlstm_fw'], ih['mlstm_bw'],
                                   ix['mlstm_fw'], ix['mlstm_bw'], m_state0, need_ctx)
        yh_x, yh_h = bidirectional(gla_scan, gla_scan, ih['hgrn_fw'], ih['hgrn_bw'],
                                   ix['hgrn_fw'], ix['hgrn_bw'], h_state0, need_ctx)
        log_gamma = jax.nn.log_sigmoid(ret_decay_logit[l].astype(f32))
        yr_x, yr_h = bidirectional(functools.partial(retention_scan, log_gamma=log_gamma[0]),
                                   functools.partial(retention_scan, log_gamma=log_gamma[1]),
                                   ih['ret'], ih['ret'], ix['ret'], ix['ret'], r_state0, need_ctx)
        mix_x = merge_branches(ym_x, yh_x, yr_x, ix['post'], head_norm_g[l], w_branch[l], w_out[l])
        x = post_norm(ALPHA * x + mod_x[2] * mix_x, post_ln_g[l, 0], post_ln_b[l, 0])
        if need_ctx:
            mix_h = merge_branches(ym_h, yh_h, yr_h, ih['post'], head_norm_g[l], w_branch[l], w_out[l])
            h = post_norm(ALPHA * h + mod_h[2] * mix_h, post_ln_g[l, 0], post_ln_b[l, 0])
        if l % 2 == 0:
            ffn = functools.partial(swiglu, w_gate=ffn_w_gate[l // 2], w_up=ffn_w_up[l // 2],
                                    w_down=ffn_w_down[l // 2])
        else:
            ffn = functools.partial(moe_swiglu, w_router=moe_w_router[l // 2], w_gate=moe_w_gate[l // 2],
                                    w_up=moe_w_up[l // 2], w_down=moe_w_down[l // 2])
        x = post_norm(ALPHA * x + mod_x[5] * ffn(modulate(x, mod_x[3], mod_x[4])),
                      post_ln_g[l, 1], post_ln_b[l, 1])
        if need_ctx:
            h = post_norm(ALPHA * h + mod_h[5] * ffn(modulate(h, mod_h[3], mod_h[4])),
                          post_ln_g[l, 1], post_ln_b[l, 1])
    return x
```

```python
import os
from contextlib import ExitStack
import numpy as np
import concourse.bass as bass
import concourse.mybir as mybir
from concourse.bass_utils import run_bass_kernel_spmd

F32 = mybir.dt.float32
BF16 = mybir.dt.bfloat16
AF = mybir.ActivationFunctionType
ALU = mybir.AluOpType

D = 2048
DEPTH = 2
SEQ = 4096
CTX = 256
T = SEQ + CTX
NT = T // 128
NCH = T // 64
BW = 1024
N_IN = 18448
FFN_DIM = 5632
EXP_DIM = 7168
N_EXP = 8
ALPHA = (2 * DEPTH) ** 0.25
EPS = 1e-6
TINY = 1e-30
RFW, RBW = 31, 32

G_MQK, G_MV, G_MZ, G_GATE, G_HQ, G_HI, G_HFF, G_HFB, G_HG, G_RQ, G_RK, G_RV, G_RG, G_MERGE = range(14)
IN_SIZES = (2048, 1024, 1024, 16, 1024, 1024, 1024, 1024, 1024, 512, 512, 1024, 1024, 6144)
IN_OFF = [0]
for _s in IN_SIZES:
    IN_OFF.append(IN_OFF[-1] + _s)

ENGS = ("pe", "act", "dve", "pool", "sp")
HMAP = {"pe": "tensor", "act": "scalar", "dve": "vector", "pool": "gpsimd", "sp": "sync"}


class Buf:
    __slots__ = ("w", "r")

    def __init__(self):
        self.w = None
        self.r = []


class TL:
    def __init__(self, t):
        self.t = t
        self.b = Buf()
        self.slot = None

    def __getitem__(self, k):
        return self.t[k]


class Ring:
    def __init__(self, tiles):
        self.tiles = tiles
        self.i = 0

    def nxt(self):
        t = self.tiles[self.i % len(self.tiles)]
        self.i += 1
        return t


class Prog:
    def __init__(self, nc, es, n_dma=88):
        self.nc = nc
        self.ops = {e: [] for e in ENGS}
        self.cnt = {e: 0 for e in ENGS}
        self.sem = {e: es.enter_context(nc.semaphore("c_" + e)) for e in ("pe", "act", "dve", "pool")}
        self.slots = [[es.enter_context(nc.semaphore("d%d" % i)), 0] for i in range(n_dma)]
        self.slot_i = 0
        self.waited = {e: {} for e in ENGS}
        self.pes = None
        self.skip = ()
        self.mute = False

    def begin(self, pes, name=None):
        self.pes = pes
        self.slot_i = 0
        self.mute = name in self.skip

    def slot(self):
        s = self.slots[self.slot_i]
        self.slot_i += 1
        return s

    def sb(self, name, shape, dt):
        self.uid = getattr(self, "uid", 0) + 1
        return TL(self.pes.enter_context(self.nc.sbuf_tensor("%s_%d" % (name, self.uid), list(shape), dt)))

    def ps(self, name, shape, dt):
        self.uid = getattr(self, "uid", 0) + 1
        return TL(self.pes.enter_context(self.nc.psum_tensor("%s_%d" % (name, self.uid), list(shape), dt)))

    def ring(self, name, shape, dt, n, psum=False):
        mk = self.ps if psum else self.sb
        return Ring([mk("%s%d" % (name, i), shape, dt) for i in range(n)])

    def _waits(self, eng, reads, writes):
        need = {}
        toks = []
        for t in reads:
            if t.b.w is not None:
                toks.append(t.b.w)
        for t in writes:
            if t.b.w is not None:
                toks.append(t.b.w)
            toks.extend(t.b.r)
        for tk in toks:
            if tk[0] == "c":
                if tk[1] == eng and eng == "pe":
                    continue
                key = tk[1]
                if need.get(key, (None, 0))[1] < tk[2]:
                    need[key] = (self.sem[tk[1]], tk[2])
            else:
                key = id(tk[1])
                if need.get(key, (None, 0))[1] < tk[2]:
                    need[key] = (tk[1][0], tk[2])
        out = []
        wd = self.waited[eng]
        for key, (s, v) in need.items():
            if wd.get(key, 0) >= v:
                continue
            wd[key] = v
            out.append((s, v))
        return out

    def _commit(self, tok, reads, writes):
        for t in reads:
            t.b.r.append(tok)
        for t in writes:
            t.b.w = tok
            t.b.r = []

    def op(self, eng, fn, reads=(), writes=()):
        if self.mute:
            return
        waits = self._waits(eng, reads, writes)
        self.cnt[eng] += 1
        tok = ("c", eng, self.cnt[eng])
        self.ops[eng].append((waits, fn, (self.sem[eng], 1)))
        self._commit(tok, reads, writes)

    def dma(self, q, out, in_, tile, reads=(), writes=(), slow=False):
        if self.mute:
            return
        if tile.slot is None or tile.slot[2] != id(self.pes):
            s = self.slot()
            tile.slot = (s[0], s, id(self.pes))
        s = tile.slot[1]
        waits = self._waits(q, reads, writes)
        s[1] += 16
        tok = ("d", s, s[1])
        if slow:
            self.ops[q].append((waits, lambda h: h.dma_start(out=out, in_=in_, allow_slow_non_contiguous=True),
                                (s[0], 16)))
        else:
            self.ops[q].append((waits, lambda h: h.dma_start(out=out, in_=in_), (s[0], 16)))
        self._commit(tok, reads, writes)

    def ld(self, tile, out, in_, q="sp", slow=False):
        self.dma(q, out, in_, tile, writes=[tile], slow=slow)

    def st(self, tile, out, in_, q="sp"):
        self.dma(q, out, in_, tile, reads=[tile])

    def end_phase(self):
        nc = self.nc
        allw = [(self.sem[e], self.cnt[e], e) for e in ("pe", "act", "dve", "pool") if self.cnt[e] > 0]
        alld = [(s[0], s[1], id(s)) for s in self.slots if s[1] > 0]
        for e in ENGS:
            wd = self.waited[e]
            ws = []
            for s, v, key in allw + alld:
                if wd.get(key, 0) >= v:
                    continue
                wd[key] = v
                ws.append((s, v))
            self.ops[e].append((ws, None, None))
        with nc.Block() as block:
            for e in ENGS:
                lst = self.ops[e]

                def body(h, lst=lst):
                    for waits, fn, inc in lst:
                        for s, v in waits:
                            h.wait_ge(s, v)
                        if fn is not None:
                            fn(h).then_inc(inc[0], inc[1])
                getattr(block, HMAP[e])(body)
        self.ops = {e: [] for e in ENGS}

    def mm(self, out, lhsT, rhs, start, stop, reads, writes):
        self.op("pe", lambda h: h.matmul(out, lhsT=lhsT, rhs=rhs, start=start, stop=stop), reads, writes)

    def tr(self, out, in_, ident, reads, writes):
        self.op("pe", lambda h: h.transpose(out, in_, ident), reads, writes)

    def act(self, out, in_, func, reads, writes, bias=None, scale=None):
        kw = {}
        if bias is not None:
            kw["bias"] = bias
        if scale is not None:
            kw["scale"] = scale
        self.op("act", lambda h: h.activation(out=out, in_=in_, func=func, **kw), reads, writes)

    def tt(self, eng, out, in0, in1, op, reads, writes):
        self.op(eng, lambda h: h.tensor_tensor(out=out, in0=in0, in1=in1, op=op), reads, writes)

    def ts(self, eng, out, in0, s1, s2, op0, op1, reads, writes, accum_out=None):
        if accum_out is not None:
            self.op(eng, lambda h: h.tensor_scalar(out=out, in0=in0, scalar1=s1, scalar2=s2, op0=op0, op1=op1,
                                                   accum_out=accum_out), reads, writes)
        elif op1 is None:
            self.op(eng, lambda h: h.tensor_scalar(out=out, in0=in0, scalar1=s1, scalar2=None, op0=op0), reads, writes)
        else:
            self.op(eng, lambda h: h.tensor_scalar(out=out, in0=in0, scalar1=s1, scalar2=s2, op0=op0, op1=op1),
                    reads, writes)

    def stt(self, eng, out, in0, scalar, in1, op0, op1, reads, writes):
        self.op(eng, lambda h: h.scalar_tensor_tensor(out=out, in0=in0, scalar=scalar, in1=in1, op0=op0, op1=op1),
                reads, writes)

    def cp(self, eng, out, in_, reads, writes):
        if eng == "act":
            self.op("act", lambda h: h.activation(out=out, in_=in_, func=AF.Copy), reads, writes)
        else:
            self.op(eng, lambda h: h.tensor_copy(out=out, in_=in_), reads, writes)

    def memset(self, eng, ap, val, writes):
        self.op(eng, lambda h: h.memset(ap, val), (), writes)


C_IDENT = 0
C_MTF = 128
C_MTB = 192
C_CSF = 256
C_CSB = 320
C_INDF = 384
C_INDB = 386
C_INDBF_L = 388
C_INDBF_M = 516
C_INDBB_L = 644
C_INDBB_M = 772
C_ML = 900
C_MR = 901
C_ONE = 902
C_N = 904


def make_consts():
    c = np.zeros((128, C_N), np.float32)
    c[:, C_IDENT:C_IDENT + 128] = np.eye(128, dtype=np.float32)
    s = np.arange(64)
    c[:64, C_MTF:C_MTF + 64] = (s[:, None] <= s[None, :])
    c[:64, C_MTB:C_MTB + 64] = (s[:, None] >= s[None, :])
    c[:64, C_CSF:C_CSF + 64] = (s[:, None] <= s[None, :]).astype(np.float32) - (s[:, None] <= RFW)
    c[:64, C_CSB:C_CSB + 64] = (s[:, None] >= s[None, :]).astype(np.float32) - (s[:, None] >= RBW)
    c[:64, C_INDF] = s > RFW
    c[:64, C_INDF + 1] = s <= RFW
    c[:64, C_INDB] = s < RBW
    c[:64, C_INDB + 1] = s >= RBW
    c[:64, C_INDBF_L:C_INDBF_L + 128] = (s > RFW)[:, None]
    c[:64, C_INDBF_M:C_INDBF_M + 128] = (s <= RFW)[:, None]
    c[:64, C_INDBB_L:C_INDBB_L + 128] = (s < RBW)[:, None]
    c[:64, C_INDBB_M:C_INDBB_M + 128] = (s >= RBW)[:, None]
    p = np.arange(128)
    c[:, C_ML] = (p % 64) != 0
    c[:, C_MR] = (p % 64) != 63
    c[:, C_ONE] = 1.0
    return c


def build_program(dbg=None, layers=DEPTH, stop_after=None, skip=(), scan_stage=9, scan_nch=None):
    nc = bass.Bass("TRN2", target_bir_lowering=False)
    dbg = dbg or []

    def din(name, shape):
        return nc.dram_tensor(name, list(shape), F32, kind="ExternalInput").ap()

    def dsc(name, shape, dt):
        kind = "ExternalOutput" if name in dbg else "Internal"
        return nc.dram_tensor(name, list(shape), dt, kind=kind).ap()

    specs = dict(
        x=[SEQ, D], c=[128, 32], ctx=[CTX, D], w_ada=[DEPTH, D, 6 * D], b_ada=[DEPTH, 6 * D],
        w_in=[DEPTH, D, N_IN], conv_w=[DEPTH, 9, D], conv_b=[DEPTH, D], mlstm_gate_b=[DEPTH, 16],
        hgrn_lb=[2, DEPTH, BW], ret_decay_logit=[DEPTH, 8], head_norm_g=[DEPTH, 3 * BW],
        w_branch=[DEPTH, 3 * BW, D], w_out=[DEPTH, D, D], post_ln_g=[DEPTH, 2, D], post_ln_b=[DEPTH, 2, D],
        ffn_w_gate=[1, D, FFN_DIM], ffn_w_up=[1, D, FFN_DIM], ffn_w_down=[1, FFN_DIM, D],
        moe_w_router=[N_EXP, D], moe_w_gate=[N_EXP, D, EXP_DIM], moe_w_up=[N_EXP, D, EXP_DIM],
        moe_w_down=[N_EXP, EXP_DIM, D], consts=[128, C_N])

    class Lazy(dict):
        def __missing__(self, k):
            self[k] = din(k, specs[k])
            return self[k]
    I = Lazy()
    build_program.in_names = I
    y_out = nc.dram_tensor("y", [SEQ, D], F32, kind="ExternalOutput").ap()

    S = dict(
        mod=dsc("mod", [DEPTH, 2, 6 * D], F32),
        uT=dsc("uT", [NT, 128, 16, 128], BF16),
        mqk=dsc("mqk", [T + 384, D], BF16),
        qk=dsc("qk", [T, D], BF16), mv=dsc("mv", [T, BW], BF16), zs=dsc("zs", [T, 3 * BW], BF16),
        gt=dsc("gt", [T, 16], F32), hq=dsc("hq", [T, BW], BF16), hi=dsc("hi", [T, BW], BF16),
        kk=dsc("kk", [2, T, BW], BF16), lf=dsc("lf", [2, T, BW], F32),
        rqk=dsc("rqk", [T, BW], BF16), rv=dsc("rv", [T, BW], BF16), mg=dsc("mg", [T, 3 * D], BF16),
        bxT=dsc("bxT", [NT, 128, 24, 128], BF16),
        mixT=dsc("mixT", [NT, 128, 16, 128], BF16),
        xmid=dsc("xmid", [T, D], F32), u2T=dsc("u2T", [NT, 128, 16, 128], BF16),
        gexp=dsc("gexp", [T, N_EXP], F32), xcur=dsc("xcur", [T, D], F32),
        ysc=dsc("ysc", [2, T, 3 * BW], F32), u2f=dsc("u2f", [T, D], F32),
    )

    def mqk_row(tok):
        return tok + 128 if tok < CTX else tok + 256

    with ExitStack() as es:
        P = Prog(nc, es)
        P.skip = tuple(skip)

        def xsrc(l, j):
            if l == 0:
                return I["ctx"][j * 128:(j + 1) * 128, :] if j < 2 else I["x"][(j - 2) * 128:(j - 1) * 128, :]
            return S["xcur"][j * 128:(j + 1) * 128, :]

        def bcast(ap_row):
            return ap_row.partition_broadcast(128)

        def load_ident(P):
            cf = P.sb("idf", [128, 128], F32)
            ib = P.sb("idb", [128, 128], BF16)
            P.ld(cf, cf[:], I["consts"][:, C_IDENT:C_IDENT + 128])
            P.cp("dve", ib[:], cf[:], [cf], [ib])
            return ib

        def ln_stats(P, x_ap, xt, st, mv, rstd, nmr, n=D):
            nchunk = n // 512
            for k in range(nchunk):
                P.op("dve", lambda h, k=k: h.bn_stats(out=st[:, k, :], in_=x_ap[:, k * 512:(k + 1) * 512]),
                     [xt], [st])
            P.op("dve", lambda h: h.bn_aggr(out=mv[:], in_=st[:, 0:nchunk, :]), [st], [mv])
            P.ts("dve", rstd[:], mv[:, 1:2], EPS, None, ALU.add, None, [mv], [rstd])
            P.act(rstd[:], rstd[:], AF.Sqrt, [rstd], [rstd])
            P.op("dve", lambda h: h.reciprocal(out=rstd[:], in_=rstd[:]), [rstd], [rstd])
            P.stt("dve", nmr[:], mv[:, 0:1], -1.0, rstd[:], ALU.mult, ALU.mult, [mv, rstd], [nmr])

        def transpose_to(P, src, src_t, nchunks, ident, ptr, dst, evac_engs=("act", "dve")):
            for g in range(0, nchunks, 8):
                pt = ptr.nxt()
                n = min(8, nchunks - g)
                for k in range(n):
                    P.tr(pt[:, k, :], src[:, (g + k) * 128:(g + k + 1) * 128], ident[:], [src_t, ident], [pt])
                P.cp(evac_engs[(g // 8) % len(evac_engs)], dst[:, g:g + n, :], pt[:, 0:n, :], [pt], [dst])

        with ExitStack() as pes:
            P.begin(pes, "0")
            cT = P.sb("cT", [128, 16, 2], F32)
            P.ld(cT, cT[:], I["c"].rearrange("p (k s) -> p k s", s=2))
            P.act(cT[:], cT[:], AF.Silu, [cT], [cT])
            wr = P.ring("wada", [128, 16, 512], F32, 2)
            pr = P.ring("pada", [2, 512], F32, 2, psum=True)
            orr = P.ring("oada", [2, 512], F32, 2)
            bada = P.sb("bada", [2, 512], F32)
            for l in range(layers):
                for nb in range(24):
                    w = wr.nxt()
                    P.ld(w, w[:], I["w_ada"][l][:, nb * 512:(nb + 1) * 512].rearrange("(k p) n -> p k n", p=128))
                    P.ld(bada, bada[:], I["b_ada"][l:l + 1, nb * 512:(nb + 1) * 512].broadcast_to([2, 512]))
                    pp = pr.nxt()
                    for k in range(16):
                        P.mm(pp[:], cT[:, k, :], w[:, k, :], k == 0, k == 15, [cT, w], [pp])
                    o = orr.nxt()
                    P.tt("dve", o[:], pp[:], bada[:], ALU.add, [pp, bada], [o])
                    P.st(o, S["mod"][l][:, nb * 512:(nb + 1) * 512], o[:])
            P.end_phase()

        for l in range(layers):
            last = (l == DEPTH - 1)
            with ExitStack() as pes:
                P.begin(pes, "A")
                ident = load_ident(P)
                vec = {}
                for s in range(2):
                    sh = P.sb("sh%d" % s, [128, D], F32)
                    sc = P.sb("sc%d" % s, [128, D], F32)
                    P.ld(sh, sh[:], bcast(S["mod"][l, s, 0:D]))
                    P.ld(sc, sc[:], bcast(S["mod"][l, s, D:2 * D]))
                    P.ts("pool", sc[:], sc[:], 1.0, None, ALU.add, None, [sc], [sc])
                    vec[s] = (sh, sc)
                xr = P.ring("xa", [128, D], F32, 3)
                xnr = P.ring("xn", [128, D], F32, 2)
                ur = P.ring("ua", [128, D], BF16, 2)
                utr = P.ring("uta", [128, 16, 128], BF16, 2)
                ptr = P.ring("pta", [128, 8, 128], BF16, 4, psum=True)
                str_ = P.ring("sta", [128, 4, 6], F32, 2)
                mvr = P.ring("mva", [128, 2], F32, 2)
                rsr = P.ring("rsa", [128, 1], F32, 2)
                nmr_ = P.ring("nma", [128, 1], F32, 2)
                for j in range(NT):
                    s = 1 if j < 2 else 0
                    sh, sc = vec[s]
                    xt = xr.nxt()
                    P.ld(xt, xt[:], xsrc(l, j))
                    st, mv, rstd, nmr = str_.nxt(), mvr.nxt(), rsr.nxt(), nmr_.nxt()
                    ln_stats(P, xt, xt, st, mv, rstd, nmr)
                    xn = xnr.nxt()
                    P.act(xn[:], xt[:], AF.Identity, [xt, rstd, nmr], [xn], bias=nmr[:], scale=rstd[:])
                    P.tt("dve", xn[:], xn[:], sc[:], ALU.mult, [xn, sc], [xn])
                    u = ur.nxt()
                    P.tt("pool", u[:], xn[:], sh[:], ALU.add, [xn, sh], [u])
                    ut = utr.nxt()
                    transpose_to(P, u, u, 16, ident, ptr, ut)
                    P.st(ut, S["uT"][j], ut[:])
                P.end_phase()
            if stop_after == ("A", l):
                break

            with ExitStack() as pes:
                P.begin(pes, "B")
                gb = P.sb("gb", [128, 16], F32)
                P.ld(gb, gb[:], bcast(I["mlstm_gate_b"][l]))
                lbt, omlt = [], []
                for d in range(2):
                    lb = P.sb("lb%d" % d, [128, BW], F32)
                    oml = P.sb("oml%d" % d, [128, BW], F32)
                    if l == 0:
                        P.memset("pool", lb[:], 0.0, [lb])
                    else:
                        P.ld(lb, lb[:], bcast(I["hgrn_lb"][d, 1]))
                        P.ld(oml, oml[:], bcast(I["hgrn_lb"][d, 0]))
                        P.tt("dve", lb[:], lb[:], oml[:], ALU.subtract, [lb, oml], [lb])
                        P.act(lb[:], lb[:], AF.Sigmoid, [lb], [lb])
                    P.ts("dve", oml[:], lb[:], -1.0, 1.0, ALU.mult, ALU.add, [lb], [oml])
                    lbt.append(lb)
                    omlt.append(oml)
                zt = P.sb("zt", [128, D], BF16)
                P.memset("pool", zt[:], 0.0, [zt])
                for r0 in (0, 128 + CTX, 256 + T):
                    P.st(zt, S["mqk"][r0:r0 + 128, :], zt[:])
                wr = P.ring("wb", [128, 16, 1024], BF16, 2)
                utr = P.ring("utb", [128, 16, 128], BF16, 3)
                pr = P.ring("pb", [128, 512], F32, 6, psum=True)
                o16 = P.ring("ob", [128, 512], BF16, 4)
                o32 = P.ring("of", [128, 512], F32, 3)
                t32 = P.ring("tf", [128, 512], F32, 3)
                g32 = P.ring("gf", [128, 16], F32, 2)
                blocks = []
                for g in range(14):
                    for c0 in range(0, IN_SIZES[g], 1024):
                        blocks.append((g, c0, min(1024, IN_SIZES[g] - c0)))
                ei = [0]

                def epilogue(g, pp, j, c0, n):
                    rows = slice(j * 128, (j + 1) * 128)
                    cs = slice(c0, c0 + n)
                    ei[0] += 1

                    def simple(dst_ap, func=None, scale=None):
                        o = o16.nxt()
                        if func is None and scale is None and ei[0] % 2 == 0:
                            P.cp("dve", o[:, 0:n], pp[:, 0:n], [pp], [o])
                        else:
                            P.act(o[:, 0:n], pp[:, 0:n], func or AF.Copy, [pp], [o], scale=scale)
                        P.st(o, dst_ap, o[:, 0:n])
                    if g == G_MQK:
                        r0 = mqk_row(j * 128)
                        simple(S["mqk"][r0:r0 + 128, cs])
                    elif g == G_MV:
                        simple(S["mv"][rows, cs])
                    elif g == G_MZ:
                        simple(S["zs"][rows, c0:c0 + n], AF.Silu)
                    elif g == G_HG:
                        simple(S["zs"][rows, BW + c0:BW + c0 + n], AF.Silu)
                    elif g == G_RG:
                        simple(S["zs"][rows, 2 * BW + c0:2 * BW + c0 + n], AF.Silu)
                    elif g == G_HQ:
                        simple(S["hq"][rows, cs], AF.Silu)
                    elif g == G_HI:
                        simple(S["hi"][rows, cs])
                    elif g == G_RQ:
                        simple(S["rqk"][rows, 0:512])
                    elif g == G_RK:
                        simple(S["rqk"][rows, 512:1024], None, 128.0 ** -0.5)
                    elif g == G_RV:
                        simple(S["rv"][rows, cs])
                    elif g == G_MERGE:
                        simple(S["mg"][rows, cs], AF.Sigmoid)
                    elif g == G_GATE:
                        o = g32.nxt()
                        P.tt("dve", o[:], pp[:, 0:16], gb[:], ALU.add, [pp, gb], [o])
                        for q0 in (4, 12):
                            P.act(o[:, q0:q0 + 4], o[:, q0:q0 + 4], AF.Sigmoid, [o], [o])
                            P.act(o[:, q0:q0 + 4], o[:, q0:q0 + 4], AF.Ln, [o], [o])
                        P.st(o, S["gt"][rows, :], o[:])
                    else:
                        d = 0 if g == G_HFF else 1
                        tmp = t32.nxt()
                        P.act(tmp[:, 0:n], pp[:, 0:n], AF.Sigmoid, [pp], [tmp])
                        P.tt("dve", tmp[:, 0:n], tmp[:, 0:n], omlt[d][:, cs], ALU.mult, [tmp, omlt[d]], [tmp])
                        P.stt("dve", tmp[:, 0:n], tmp[:, 0:n], TINY, lbt[d][:, cs], ALU.max, ALU.add,
                              [tmp, lbt[d]], [tmp])
                        o = o32.nxt()
                        P.act(o[:, 0:n], tmp[:, 0:n], AF.Ln, [tmp], [o])
                        P.st(o, S["lf"][d, rows, cs], o[:, 0:n])
                        ob = o16.nxt()
                        P.ts("pool", ob[:, 0:n], tmp[:, 0:n], -1.0, 1.0, ALU.mult, ALU.add, [tmp], [ob])
                        P.st(ob, S["kk"][d, rows, cs], ob[:, 0:n])

                for (g, c0, n) in blocks:
                    w = wr.nxt()
                    col = IN_OFF[g] + c0
                    P.ld(w, w[:, :, 0:n], I["w_in"][l][:, col:col + n].rearrange("(k p) n -> p k n", p=128), q="pool")
                    for j in range(NT):
                        ut = utr.nxt()
                        P.ld(ut, ut[:], S["uT"][j])
                        for h0 in range(0, n, 512):
                            hn = min(512, n - h0)
                            pp = pr.nxt()
                            for k in range(16):
                                P.mm(pp[:, 0:hn], ut[:, k, :], w[:, k, h0:h0 + hn], k == 0, k == 15, [ut, w], [pp])
                            epilogue(g, pp, j, c0 + h0, hn)
                P.end_phase()
            if stop_after == ("B", l):
                break

            tiles_post = list(range(2, NT)) if last else list(range(NT))

            with ExitStack() as pes:
                P.begin(pes, "C")
                cm = P.sb("cm", [128, 2], F32)
                P.ld(cm, cm[:], I["consts"][:, C_ML:C_ML + 2])
                cb = P.sb("cb", [128, D], F32)
                P.ld(cb, cb[:], bcast(I["conv_b"][l]))
                wts = []
                for tap in range(9):
                    w = P.sb("wm%d" % tap, [128, D], F32)
                    P.ld(w, w[:], bcast(I["conv_w"][l, tap]))
                    wts.append(w)
                xsr = P.ring("xs", [128, D], BF16, 4)
                accr = P.ring("acc", [128, D], F32, 2)
                tmpr = P.ring("ctmp", [128, D], F32, 2)
                outr = P.ring("cout", [128, D], BF16, 2)

                def conv_tile(j, taps):
                    acc = accr.nxt()
                    r0 = mqk_row(j * 128)
                    for i, (off, w) in enumerate(taps):
                        xs = xsr.nxt()
                        P.ld(xs, xs[:], S["mqk"][r0 + off:r0 + off + 128, :])
                        if i == 0:
                            P.tt("pool", acc[:], xs[:], w[:], ALU.mult, [xs, w], [acc])
                        else:
                            tmp = tmpr.nxt()
                            P.tt("pool", tmp[:], xs[:], w[:], ALU.mult, [xs, w], [tmp])
                            P.tt("dve", acc[:], acc[:], tmp[:], ALU.add, [acc, tmp], [acc])
                    P.tt("dve", acc[:], acc[:], cb[:], ALU.add, [acc, cb], [acc])
                    o = outr.nxt()
                    P.act(o[:], acc[:], AF.Silu, [acc], [o])
                    P.ts("dve", o[:, BW:2 * BW], o[:, BW:2 * BW], 0.0625, None, ALU.mult, None, [o], [o])
                    P.st(o, S["qk"][j * 128:(j + 1) * 128, :], o[:])

                for j in range(2):
                    conv_tile(j, [(-1, wts[3]), (0, wts[4]), (1, wts[5])])
                for tap in (0, 3, 6):
                    P.ts("dve", wts[tap][:], wts[tap][:], cm[:, 0:1], None, ALU.mult, None, [wts[tap], cm], [wts[tap]])
                for tap in (2, 5, 8):
                    P.ts("dve", wts[tap][:], wts[tap][:], cm[:, 1:2], None, ALU.mult, None, [wts[tap], cm], [wts[tap]])
                for j in range(2, NT):
                    conv_tile(j, [((tap // 3 - 1) * 64 + (tap % 3 - 1), wts[tap]) for tap in range(9)])
                P.end_phase()
            if stop_after == ("C", l):
                break

            def scan_pass(d):
                with ExitStack() as pes:
                    P.begin(pes, "S%d" % d)
                    ident = load_ident(P)
                    cst = P.sb("cst", [64, 772], F32)
                    P.ld(cst, cst[:], I["consts"][0:64, 128:900])

                    def cc(c0, n):
                        return cst[:, c0 - 128:c0 - 128 + n]
                    maskT = cc(C_MTF if d == 0 else C_MTB, 64)
                    CS = cc(C_CSF if d == 0 else C_CSB, 64)
                    IND = cc(C_INDF if d == 0 else C_INDB, 2)
                    INDL = cc(C_INDBF_L if d == 0 else C_INDBB_L, 128)
                    INDM = cc(C_INDBF_M if d == 0 else C_INDBB_M, 128)
                    lg = P.sb("lg", [64, 4], F32)
                    P.ld(lg, lg[:], I["ret_decay_logit"][l, d * 4:(d + 1) * 4].partition_broadcast(64))
                    P.act(lg[:], lg[:], AF.Sigmoid, [lg], [lg])
                    P.act(lg[:], lg[:], AF.Ln, [lg], [lg])
                    lfsr = P.ring("lfs", [64, 8], F32, 2)
                    for t_ in lfsr.tiles:
                        P.cp("dve", t_[:, 4:8], lg[:], [lg], [t_])
                    Sm = P.sb("Sm", [128, 4, 514], F32)
                    Smb = P.sb("Smb", [128, 4, 514], BF16)
                    Sh = P.sb("Sh", [128, 8, 128], F32)
                    Shb = P.sb("Shb", [128, 8, 128], BF16)
                    Sr = P.sb("Sr", [128, 4, 256], F32)
                    Srb = P.sb("Srb", [128, 4, 256], BF16)
                    elp = P.sb("elp", [128, 16], F32)
                    for t_ in (Sm, Smb, Sh, Shb, Sr, Srb, elp):
                        P.memset("pool", t_[:], 0.0, [t_])
                    qkr = P.ring("sqk", [64, D], BF16, 2)
                    vmr = P.ring("svm", [64, 4, 257], BF16, 2)
                    for t_ in vmr.tiles:
                        P.memset("pool", t_[:, :, 256:257], 1.0, [t_])
                    gtr = P.ring("sgt", [64, 16], F32, 2)
                    hqr = P.ring("shq", [64, BW], BF16, 2)
                    hir = P.ring("shi", [64, BW], BF16, 2)
                    kkr = P.ring("skk", [64, BW], BF16, 2)
                    lfr = P.ring("slf", [64, BW], F32, 2)
                    rqr = P.ring("srq", [64, BW], BF16, 2)
                    rvr = P.ring("srv", [64, BW], BF16, 2)
                    eqr = P.ring("eqs", [64, 8], F32, 2)
                    ekr = P.ring("eks", [64, 8], F32, 2)
                    exr = P.ring("exs", [128, 16], F32, 2)
                    g16r = P.ring("g16", [128, 16], F32, 2)
                    e1r = P.ring("e1", [64, BW], F32, 2)
                    e2r = P.ring("e2", [64, BW], F32, 2)
                    qar = P.ring("QA", [64, 2560], BF16, 2)
                    kar = P.ring("KA", [64, 2560], BF16, 2)
                    qtr = P.ring("QT", [128, 20, 64], BF16, 2)
                    ktr = P.ring("KT", [128, 20, 64], BF16, 2)
                    ptr_ = P.ring("pT", [64, 16, 64], BF16, 2)
                    for t_ in ptr_.tiles:
                        P.memset("pool", t_[:], 0.0, [t_])
                    mfull = P.sb("mfull", [64, 8, 64], F32)
                    P.cp("dve", mfull[:], maskT.unsqueeze(1).to_broadcast([64, 8, 64]), [cst], [mfull])
                    yr_ = P.ring("Y", [64, 3 * BW], F32, 2)
                    dnr = P.ring("dn", [64, 1], F32, 4)
                    tpr = P.ring("tps", [128, 16, 64], BF16, 2, psum=True)
                    atr = P.ring("ats", [64, 8, 64], F32, 2, psum=True)
                    outr = P.ring("outs", [128, 512], F32, 2, psum=True)
                    dsr = P.ring("dss", [128, 512], F32, 1, psum=True)
                    gpr = P.ring("gps", [128, 64], F32, 1, psum=True)
                    units = [[2 * h, 2 * h + 1] for h in range(4)] + [[8 + h] for h in range(8)] + \
                            [[16 + h] for h in range(4)]
                    if d == 0:
                        order = list(range(NCH))
                    else:
                        order = [3, 2, 1, 0] + list(range(NCH - 1, 3, -1))
                    b3 = lambda ap, h, n: ap.unsqueeze(2).to_broadcast([ap.shape[0], h, n])
                    if scan_nch:
                        order = order[:scan_nch]
                    for ci in order:
                        r = slice(ci * 64, ci * 64 + 64)
                        qk, vm, gt, hq, hi = qkr.nxt(), vmr.nxt(), gtr.nxt(), hqr.nxt(), hir.nxt()
                        kk, lf, rq, rv = kkr.nxt(), lfr.nxt(), rqr.nxt(), rvr.nxt()
                        P.ld(gt, gt[:], S["gt"][r, :])
                        P.ld(lf, lf[:], S["lf"][d, r, :])
                        P.ld(qk, qk[:], S["qk"][r, :])
                        P.ld(hq, hq[:], S["hq"][r, :])
                        P.ld(kk, kk[:], S["kk"][d, r, :])
                        P.ld(rq, rq[:], S["rqk"][r, :])
                        P.ld(vm, vm[:, :, 0:256], S["mv"][r, :].rearrange("p (h c) -> p h c", h=4))
                        P.ld(hi, hi[:], S["hi"][r, :])
                        P.ld(rv, rv[:], S["rv"][r, :])
                        lfs = lfsr.nxt()
                        P.cp("dve", lfs[:, 0:4], gt[:, 8 * d + 4:8 * d + 8], [gt], [lfs])
                        gp = gpr.nxt()
                        P.mm(gp[0:64, 32:40], CS, lfs[:], True, True, [cst, lfs], [gp])
                        P.mm(gp[:, 0:8], INDL, lfs[:], True, True, [cst, lfs], [gp])
                        P.mm(gp[:, 8:16], INDM, lfs[:], True, True, [cst, lfs], [gp])
                        for h in range(8):
                            P.mm(gp[:, 16 + 2 * h:18 + 2 * h], lf[:, h * 128:(h + 1) * 128], IND, True, True,
                                 [lf, cst], [gp])
                        eqs, eks, ex, g16 = eqr.nxt(), ekr.nxt(), exr.nxt(), g16r.nxt()
                        P.act(eqs[:], gp[0:64, 32:40], AF.Exp, [gp], [eqs])
                        P.tt("dve", eks[:, 0:4], gt[:, 8 * d:8 * d + 4], gp[0:64, 32:36], ALU.subtract, [gt, gp], [eks])
                        P.act(eks[:, 0:4], eks[:, 0:4], AF.Exp, [eks], [eks])
                        P.act(eks[:, 4:8], gp[0:64, 36:40], AF.Exp, [gp], [eks], scale=-1.0)
                        gph = gp[:, 16:32].rearrange("p (h t) -> p h t", t=2)
                        P.tt("dve", ex[:, 0:4], gp[:, 8:12], elp[:, 0:4], ALU.add, [gp, elp], [ex])
                        P.tt("dve", ex[:, 4:12], gph[:, :, 1], elp[:, 4:12], ALU.add, [gp, elp], [ex])
                        P.tt("dve", ex[:, 12:16], gp[:, 12:16], elp[:, 12:16], ALU.add, [gp, elp], [ex])
                        P.act(g16[:], ex[:], AF.Exp, [ex], [g16])
                        P.cp("dve", elp[:, 0:4], gp[:, 0:4], [gp], [elp])
                        P.cp("dve", elp[:, 4:12], gph[:, :, 0], [gp], [elp])
                        P.cp("dve", elp[:, 12:16], gp[:, 4:8], [gp], [elp])
                        if scan_stage < 2:
                            continue
                        P.tt("pool", Sm[:], Sm[:], b3(g16[:, 0:4], 4, 514), ALU.mult, [Sm, g16], [Sm])
                        P.cp("act", Smb[:], Sm[:], [Sm], [Smb])
                        P.tt("pool", Sh[:], Sh[:], b3(g16[:, 4:12], 8, 128), ALU.mult, [Sh, g16], [Sh])
                        P.cp("act", Shb[:], Sh[:], [Sh], [Shb])
                        P.tt("pool", Sr[:], Sr[:], b3(g16[:, 12:16], 4, 256), ALU.mult, [Sr, g16], [Sr])
                        P.cp("act", Srb[:], Sr[:], [Sr], [Srb])
                        if scan_stage < 3:
                            continue
                        bh = [outr.nxt(), outr.nxt()]
                        e1, e2 = e1r.nxt(), e2r.nxt()
                        for k in range(2):
                            P.mm(bh[k][0:64, :], CS, lf[:, k * 512:(k + 1) * 512], True, True, [cst, lf], [bh[k]])
                            P.act(e1[:, k * 512:(k + 1) * 512], bh[k][0:64, :], AF.Exp, [bh[k]], [e1])
                            P.act(e2[:, k * 512:(k + 1) * 512], bh[k][0:64, :], AF.Exp, [bh[k]], [e2], scale=-1.0)
                        QA, KA = qar.nxt(), kar.nxt()
                        v3 = lambda ap, h: ap.rearrange("p (h c) -> p h c", h=h)
                        P.tt("dve", QA[:, 1024:2048], hq[:], e1[:], ALU.mult, [hq, e1], [QA])
                        P.tt("pool", KA[:, 1024:2048], kk[:], e2[:], ALU.mult, [kk, e2], [KA])
                        P.tt("dve", v3(QA[:, 0:1024], 4), v3(qk[:, 0:1024], 4), b3(eqs[:, 0:4], 4, 256), ALU.mult,
                             [qk, eqs], [QA])
                        P.tt("pool", v3(KA[:, 0:1024], 4), v3(qk[:, 1024:2048], 4), b3(eks[:, 0:4], 4, 256), ALU.mult,
                             [qk, eks], [KA])
                        P.tt("dve", v3(QA[:, 2048:2560], 4), v3(rq[:, 0:512], 4), b3(eqs[:, 4:8], 4, 128), ALU.mult,
                             [rq, eqs], [QA])
                        P.tt("pool", v3(KA[:, 2048:2560], 4), v3(rq[:, 512:1024], 4), b3(eks[:, 4:8], 4, 128), ALU.mult,
                             [rq, eks], [KA])
                        if scan_stage < 4:
                            continue
                        QT, KT = qtr.nxt(), ktr.nxt()
                        ei_ = 0
                        for (src, dst) in ((QA, QT), (KA, KT)):
                            for g0 in (0, 16):
                                pt = tpr.nxt()
                                n = min(16, 20 - g0)
                                for k in range(n):
                                    u = g0 + k
                                    P.tr(pt[:, k, :], src[:, u * 128:(u + 1) * 128], ident[0:64, 0:64], [src, ident], [pt])
                                P.cp(("act", "dve")[ei_ % 2], dst[:, g0:g0 + n, :], pt[:, 0:n, :], [pt], [dst])
                                ei_ += 1
                        if scan_stage < 5:
                            continue
                        at = [atr.nxt(), atr.nxt()]
                        for hd in range(16):
                            a = at[hd // 8]
                            us = units[hd]
                            for i, u in enumerate(us):
                                P.mm(a[:, hd % 8, :], KT[:, u, :], QT[:, u, :], i == 0, i == len(us) - 1, [KT, QT], [a])
                        pT = ptr_.nxt()
                        for k in range(2):
                            P.op("dve", lambda hh, pT=pT, a=at[k], k=k: hh.copy_predicated(
                                out=pT[:, k * 8:(k + 1) * 8, :], mask=mfull[:].bitcast(mybir.dt.int32), data=a[:]),
                                [at[k], mfull], [pT])
                        if scan_stage < 6:
                            continue
                        Y = yr_.nxt()
                        for h in range(4):
                            po = outr.nxt()
                            P.mm(po[0:64, 0:257], QT[:, 2 * h, :], Smb[:, h, 0:257], True, False, [QT, Smb], [po])
                            P.mm(po[0:64, 0:257], QT[:, 2 * h + 1, :], Smb[:, h, 257:514], False, False, [QT, Smb], [po])
                            P.mm(po[0:64, 0:257], pT[:, h, :], vm[:, h, :], False, True, [pT, vm], [po])
                            dn = dnr.nxt()
                            P.act(dn[:], po[0:64, 256:257], AF.Abs, [po], [dn])
                            P.ts("dve", dn[:], dn[:], 1.0, None, ALU.max, None, [dn], [dn])
                            P.op("dve", lambda hh, dn=dn: hh.reciprocal(out=dn[:], in_=dn[:]), [dn], [dn])
                            P.ts("dve", Y[:, h * 256:(h + 1) * 256], po[0:64, 0:256], dn[:, 0:1], None, ALU.mult, None,
                                 [po, dn], [Y])
                        for k in range(2):
                            po = outr.nxt()
                            for hh in range(4):
                                h = k * 4 + hh
                                cs = slice(hh * 128, (hh + 1) * 128)
                                P.mm(po[0:64, cs], QT[:, 8 + h, :], Shb[:, h, :], True, False, [QT, Shb], [po])
                                P.mm(po[0:64, cs], pT[:, 4 + h, :], hi[:, h * 128:(h + 1) * 128], False, True, [pT, hi], [po])
                            P.cp("act", Y[:, BW + k * 512:BW + (k + 1) * 512], po[0:64, :], [po], [Y])
                        for k in range(2):
                            po = outr.nxt()
                            for hh in range(2):
                                h = k * 2 + hh
                                cs = slice(hh * 256, (hh + 1) * 256)
                                P.mm(po[0:64, cs], QT[:, 16 + h, :], Srb[:, h, :], True, False, [QT, Srb], [po])
                                P.mm(po[0:64, cs], pT[:, 12 + h, :], rv[:, h * 256:(h + 1) * 256], False, True, [pT, rv], [po])
                            P.cp("dve", Y[:, 2 * BW + k * 512:2 * BW + (k + 1) * 512], po[0:64, :], [po], [Y])
                        P.st(Y, S["ysc"][d, r, :], Y[:])
                        if scan_stage < 7:
                            continue
                        for h in range(4):
                            for i in range(2):
                                u = 2 * h + i
                                pd = dsr.nxt()
                                P.mm(pd[:, 0:257], KA[:, u * 128:(u + 1) * 128], vm[:, h, :], True, True, [KA, vm], [pd])
                                P.tt("dve", Sm[:, h, i * 257:(i + 1) * 257], Sm[:, h, i * 257:(i + 1) * 257], pd[:, 0:257],
                                     ALU.add, [Sm, pd], [Sm])
                        for k in range(2):
                            pd = dsr.nxt()
                            for hh in range(4):
                                h = k * 4 + hh
                                P.mm(pd[:, hh * 128:(hh + 1) * 128], KA[:, (8 + h) * 128:(9 + h) * 128],
                                     hi[:, h * 128:(h + 1) * 128], True, True, [KA, hi], [pd])
                            P.tt("dve", Sh[:, k * 4:(k + 1) * 4, :], Sh[:, k * 4:(k + 1) * 4, :], v3(pd[:], 4), ALU.add,
                                 [Sh, pd], [Sh])
                        for k in range(2):
                            pd = dsr.nxt()
                            for hh in range(2):
                                h = k * 2 + hh
                                P.mm(pd[:, hh * 256:(hh + 1) * 256], KA[:, (16 + h) * 128:(17 + h) * 128],
                                     rv[:, h * 256:(h + 1) * 256], True, True, [KA, rv], [pd])
                            P.tt("dve", Sr[:, k * 2:(k + 1) * 2, :], Sr[:, k * 2:(k + 1) * 2, :], v3(pd[:], 2), ALU.add,
                                 [Sr, pd], [Sr])
                    P.end_phase()

            scan_pass(0)
            if stop_after == ("S0", l):
                break
            scan_pass(1)
            if stop_after == ("S1", l):
                break

            with ExitStack() as pes:
                P.begin(pes, "F")
                ident = load_ident(P)
                ng = P.sb("ng", [128, 3 * BW], F32)
                P.ld(ng, ng[:], bcast(I["head_norm_g"][l]))
                yar = P.ring("ya", [128, 3 * BW], F32, 2)
                ybr = P.ring("yb", [128, 3 * BW], F32, 2)
                zr = P.ring("fz", [128, 3 * BW], BF16, 2)
                sqr = P.ring("fsq", [128, 3 * BW], F32, 1)
                gzr = P.ring("fgz", [128, 3 * BW], F32, 1)
                bxr = P.ring("fbx", [128, 3 * BW], BF16, 2)
                btr = P.ring("fbt", [128, 24, 128], BF16, 2)
                mur = P.ring("fmu", [128, 16], F32, 2)
                ssr = P.ring("fss", [128, 16], F32, 2)
                ptr = P.ring("ptf", [128, 8, 128], BF16, 4, psum=True)
                b3 = lambda ap, h, n: ap.unsqueeze(2).to_broadcast([ap.shape[0], h, n])
                v3 = lambda ap, h: ap.rearrange("p (h c) -> p h c", h=h)
                for j in tiles_post:
                    rows = slice(j * 128, (j + 1) * 128)
                    ya, yb, z = yar.nxt(), ybr.nxt(), zr.nxt()
                    P.ld(ya, ya[:], S["ysc"][0, rows, :])
                    P.ld(yb, yb[:], S["ysc"][1, rows, :])
                    P.ld(z, z[:], S["zs"][rows, :])
                    P.tt("dve", ya[:], ya[:], yb[:], ALU.add, [ya, yb], [ya])
                    mu, ss = mur.nxt(), ssr.nxt()
                    for (c0, mc) in ((0, 0), (2 * BW, 4)):
                        y3 = v3(ya[:, c0:c0 + BW], 4)
                        P.op("dve", lambda hh, y3=y3, mu=mu, mc=mc: hh.reduce_sum(out=mu[:, mc:mc + 4], in_=y3,
                                                                               axis=mybir.AxisListType.X), [ya], [mu])
                        P.ts("dve", mu[:, mc:mc + 4], mu[:, mc:mc + 4], -1.0 / 256, None, ALU.mult, None, [mu], [mu])
                        P.tt("dve", y3, y3, b3(mu[:, mc:mc + 4], 4, 256), ALU.add, [ya, mu], [ya])
                    sq = sqr.nxt()
                    P.tt("pool", sq[:], ya[:], ya[:], ALU.mult, [ya], [sq])
                    P.op("dve", lambda hh, sq=sq, ss=ss: hh.reduce_sum(out=ss[:, 0:4], in_=v3(sq[:, 0:BW], 4),
                                                                     axis=mybir.AxisListType.X), [sq], [ss])
                    P.op("dve", lambda hh, sq=sq, ss=ss: hh.reduce_sum(out=ss[:, 4:12], in_=v3(sq[:, BW:2 * BW], 8),
                                                                     axis=mybir.AxisListType.X), [sq], [ss])
                    P.op("dve", lambda hh, sq=sq, ss=ss: hh.reduce_sum(out=ss[:, 12:16], in_=v3(sq[:, 2 * BW:3 * BW], 4),
                                                                     axis=mybir.AxisListType.X), [sq], [ss])
                    P.ts("dve", ss[:, 0:4], ss[:, 0:4], 1.0 / 256, EPS, ALU.mult, ALU.add, [ss], [ss])
                    P.ts("dve", ss[:, 4:12], ss[:, 4:12], 1.0 / 128, EPS, ALU.mult, ALU.add, [ss], [ss])
                    P.ts("dve", ss[:, 12:16], ss[:, 12:16], 1.0 / 256, EPS, ALU.mult, ALU.add, [ss], [ss])
                    P.act(ss[:], ss[:], AF.Sqrt, [ss], [ss])
                    P.op("dve", lambda hh, ss=ss: hh.reciprocal(out=ss[:], in_=ss[:]), [ss], [ss])
                    P.tt("dve", v3(ya[:, 0:BW], 4), v3(ya[:, 0:BW], 4), b3(ss[:, 0:4], 4, 256), ALU.mult, [ya, ss], [ya])
                    P.tt("dve", v3(ya[:, BW:2 * BW], 8), v3(ya[:, BW:2 * BW], 8), b3(ss[:, 4:12], 8, 128), ALU.mult,
                         [ya, ss], [ya])
                    P.tt("dve", v3(ya[:, 2 * BW:3 * BW], 4), v3(ya[:, 2 * BW:3 * BW], 4), b3(ss[:, 12:16], 4, 256),
                         ALU.mult, [ya, ss], [ya])
                    gz = gzr.nxt()
                    P.tt("pool", gz[:], z[:], ng[:], ALU.mult, [z, ng], [gz])
                    bx = bxr.nxt()
                    P.tt("pool", bx[:], ya[:], gz[:], ALU.mult, [ya, gz], [bx])
                    bt = btr.nxt()
                    transpose_to(P, bx, bx, 24, ident, ptr, bt)
                    P.st(bt, S["bxT"][j], bt[:])
                P.end_phase()
            if stop_after == ("F", l):
                break

            with ExitStack() as pes:
                P.begin(pes, "D1")
                ident = load_ident(P)
                wbs = []
                for n in range(3):
                    wb = P.sb("wbr%d" % n, [128, 8, D], BF16)
                    P.ld(wb, wb[:], I["w_branch"][l][n * BW:(n + 1) * BW, :].rearrange("(c p) d -> p c d", p=128),
                         q="pool")
                    wbs.append(wb)
                btr = P.ring("dbt", [128, 24, 128], BF16, 2)
                mgr = P.ring("dmg", [128, 3 * D], BF16, 2)
                mixr = P.ring("dmix", [128, D], F32, 2)
                mixbr = P.ring("dmixb", [128, D], BF16, 2)
                tmpr = P.ring("dtmp", [128, 512], F32, 3)
                mtr = P.ring("dmt", [128, 16, 128], BF16, 2)
                pr = P.ring("pd1", [128, 512], F32, 4, psum=True)
                ptr = P.ring("ptd1", [128, 8, 128], BF16, 2, psum=True)
                for j in tiles_post:
                    rows = slice(j * 128, (j + 1) * 128)
                    bt, mg = btr.nxt(), mgr.nxt()
                    P.ld(bt, bt[:], S["bxT"][j])
                    P.ld(mg, mg[:], S["mg"][rows, :])
                    mixed, mixb = mixr.nxt(), mixbr.nxt()
                    for nb in range(4):
                        nbs = slice(nb * 512, (nb + 1) * 512)
                        for n in range(3):
                            pp = pr.nxt()
                            for c in range(8):
                                P.mm(pp[:], bt[:, n * 8 + c, :], wbs[n][:, c, nbs], c == 0, c == 7, [bt, wbs[n]], [pp])
                            gsl = mg[:, n * D + nb * 512:n * D + (nb + 1) * 512]
                            if n == 0:
                                P.tt("dve", mixed[:, nbs], pp[:], gsl, ALU.mult, [pp, mg], [mixed])
                            else:
                                tmp = tmpr.nxt()
                                P.tt("dve", tmp[:], pp[:], gsl, ALU.mult, [pp, mg], [tmp])
                                dst = mixb if n == 2 else mixed
                                P.tt("pool", dst[:, nbs], mixed[:, nbs], tmp[:], ALU.add, [mixed, tmp], [dst])
                    mt = mtr.nxt()
                    transpose_to(P, mixb, mixb, 16, ident, ptr, mt)
                    P.st(mt, S["mixT"][j], mt[:])
                P.end_phase()
            if stop_after == ("D1", l):
                break

            moe_layer = (l % 2 == 1)
            with ExitStack() as pes:
                P.begin(pes, "D2")
                ident = load_ident(P)
                wo = P.sb("wo", [128, 16, D], BF16)
                P.ld(wo, wo[:], I["w_out"][l].rearrange("(c p) d -> p c d", p=128), q="pool")
                g1 = P.sb("g1", [128, D], F32)
                b1 = P.sb("b1", [128, D], F32)
                P.ld(g1, g1[:], bcast(I["post_ln_g"][l, 0]))
                P.ld(b1, b1[:], bcast(I["post_ln_b"][l, 0]))
                gate1 = P.sb("gate1", [128, D], F32)
                sc2 = P.sb("sc2", [128, D], F32)
                sh2 = P.sb("sh2", [128, D], F32)
                mtr = P.ring("emt", [128, 16, 128], BF16, 2)
                xr = P.ring("ex", [128, D], F32, 2)
                zr = P.ring("ez", [128, D], F32, 2)
                xmr = P.ring("exm", [128, D], F32, 2)
                xnr = P.ring("exn", [128, D], F32, 1)
                ufr = P.ring("euf", [128, D], F32, 1)
                ubr = P.ring("eub", [128, D], BF16, 2)
                utr = P.ring("eut", [128, 16, 128], BF16, 2)
                tmpr = P.ring("etmp", [128, 512], F32, 3)
                str_ = P.ring("est", [128, 4, 6], F32, 2)
                mvr = P.ring("emv", [128, 2], F32, 2)
                rsr = P.ring("ers", [128, 1], F32, 2)
                nmr_ = P.ring("enm", [128, 1], F32, 2)
                pr = P.ring("pd2", [128, 512], F32, 4, psum=True)
                ptr = P.ring("ptd2", [128, 8, 128], BF16, 2, psum=True)
                cur_s = None
                for j in tiles_post:
                    s = 1 if j < 2 else 0
                    if s != cur_s:
                        P.ld(gate1, gate1[:], bcast(S["mod"][l, s, 2 * D:3 * D]))
                        P.ld(sh2, sh2[:], bcast(S["mod"][l, s, 3 * D:4 * D]))
                        P.ld(sc2, sc2[:], bcast(S["mod"][l, s, 4 * D:5 * D]))
                        P.ts("pool", sc2[:], sc2[:], 1.0, 1.0, ALU.add, ALU.mult, [sc2], [sc2])
                        cur_s = s
                    rows = slice(j * 128, (j + 1) * 128)
                    mt, xt, z = mtr.nxt(), xr.nxt(), zr.nxt()
                    P.ld(mt, mt[:], S["mixT"][j])
                    P.ld(xt, xt[:], xsrc(l, j))
                    for nb in range(4):
                        nbs = slice(nb * 512, (nb + 1) * 512)
                        pp = pr.nxt()
                        for c in range(16):
                            P.mm(pp[:], mt[:, c, :], wo[:, c, nbs], c == 0, c == 15, [mt, wo], [pp])
                        tmp = tmpr.nxt()
                        P.tt("dve", tmp[:], pp[:], gate1[:, nbs], ALU.mult, [pp, gate1], [tmp])
                        P.stt("dve", z[:, nbs], xt[:, nbs], ALPHA, tmp[:], ALU.mult, ALU.add, [xt, tmp], [z])
                    st, mv, rstd, nmr = str_.nxt(), mvr.nxt(), rsr.nxt(), nmr_.nxt()
                    ln_stats(P, z, z, st, mv, rstd, nmr)
                    xm = xmr.nxt()
                    P.act(xm[:], z[:], AF.Identity, [z, rstd, nmr], [xm], bias=nmr[:], scale=rstd[:])
                    P.tt("dve", xm[:], xm[:], g1[:], ALU.mult, [xm, g1], [xm])
                    P.tt("pool", xm[:], xm[:], b1[:], ALU.add, [xm, b1], [xm])
                    P.st(xm, S["xmid"][rows, :], xm[:])
                    st, mv, rstd, nmr = str_.nxt(), mvr.nxt(), rsr.nxt(), nmr_.nxt()
                    ln_stats(P, xm, xm, st, mv, rstd, nmr)
                    xn = xnr.nxt()
                    P.act(xn[:], xm[:], AF.Identity, [xm, rstd, nmr], [xn], bias=nmr[:], scale=rstd[:])
                    P.tt("dve", xn[:], xn[:], sc2[:], ALU.mult, [xn, sc2], [xn])
                    ub = ubr.nxt()
                    if moe_layer:
                        uf = ufr.nxt()
                        P.tt("pool", uf[:], xn[:], sh2[:], ALU.add, [xn, sh2], [uf])
                        P.st(uf, S["u2f"][rows, :], uf[:])
                        P.cp("act", ub[:], uf[:], [uf], [ub])
                    else:
                        P.tt("pool", ub[:], xn[:], sh2[:], ALU.add, [xn, sh2], [ub])
                    ut = utr.nxt()
                    transpose_to(P, ub, ub, 16, ident, ptr, ut)
                    P.st(ut, S["u2T"][j], ut[:])
                P.end_phase()
            if stop_after == ("D2", l):
                break

            if moe_layer:
                with ExitStack() as pes:
                    P.begin(pes, "R")
                    wrt = []
                    for e in range(N_EXP):
                        w = P.sb("wr%d" % e, [128, D], F32)
                        P.ld(w, w[:], bcast(I["moe_w_router"][e]))
                        wrt.append(w)
                    ufr = P.ring("ruf", [128, D], F32, 3)
                    jr = P.ring("rj", [128, D], F32, 2)
                    lgr = P.ring("rlg", [128, 8], F32, 2)
                    t8r = P.ring("rt8", [128, 8], F32, 2)
                    m8r = P.ring("rm8", [128, 8], F32, 2)
                    n8r = P.ring("rn8", [128, 8], F32, 2)
                    g8r = P.ring("rg8", [128, 8], F32, 2)
                    s1r = P.ring("rs1", [128, 4], F32, 2)
                    for j in tiles_post:
                        rows = slice(j * 128, (j + 1) * 128)
                        uf = ufr.nxt()
                        P.ld(uf, uf[:], S["u2f"][rows, :])
                        lg = lgr.nxt()
                        for e in range(N_EXP):
                            jk = jr.nxt()
                            P.tt("pool", jk[:], uf[:], wrt[e][:], ALU.mult, [uf, wrt[e]], [jk])
                            P.op("dve", lambda hh, jk=jk, e=e, lg=lg: hh.reduce_sum(
                                out=lg[:, e:e + 1], in_=jk[:], axis=mybir.AxisListType.X), [jk], [lg])
                        s1, t8, m8, n8, g8 = s1r.nxt(), t8r.nxt(), m8r.nxt(), n8r.nxt(), g8r.nxt()
                        AXX = mybir.AxisListType.X
                        P.op("dve", lambda hh, s1=s1, lg=lg: hh.reduce_max(out=s1[:, 0:1], in_=lg[:], axis=AXX), [lg], [s1])
                        P.ts("dve", m8[:], lg[:], s1[:, 0:1], None, ALU.is_equal, None, [lg, s1], [m8])
                        P.stt("dve", t8[:], m8[:], -1e30, lg[:], ALU.mult, ALU.add, [m8, lg], [t8])
                        P.op("dve", lambda hh, s1=s1, t8=t8: hh.reduce_max(out=s1[:, 1:2], in_=t8[:], axis=AXX), [t8], [s1])
                        P.ts("dve", n8[:], t8[:], s1[:, 1:2], None, ALU.is_equal, None, [t8, s1], [n8])
                        P.tt("dve", s1[:, 2:3], s1[:, 1:2], s1[:, 0:1], ALU.subtract, [s1], [s1])
                        P.act(s1[:, 2:3], s1[:, 2:3], AF.Sigmoid, [s1], [s1])
                        P.ts("dve", s1[:, 3:4], s1[:, 2:3], -1.0, 1.0, ALU.mult, ALU.add, [s1], [s1])
                        P.ts("dve", g8[:], m8[:], s1[:, 3:4], None, ALU.mult, None, [m8, s1], [g8])
                        P.stt("dve", g8[:], n8[:], s1[:, 2:3], g8[:], ALU.mult, ALU.add, [n8, s1, g8], [g8])
                        P.st(g8, S["gexp"][rows, :], g8[:])
                    P.end_phase()
            if stop_after == ("R", l):
                break

            with ExitStack() as pes:
                P.begin(pes, "E")
                g2 = P.sb("g2", [128, D], F32)
                b2 = P.sb("b2", [128, D], F32)
                P.ld(g2, g2[:], bcast(I["post_ln_g"][l, 1]))
                P.ld(b2, b2[:], bcast(I["post_ln_b"][l, 1]))
                gate2 = P.sb("gate2", [128, D], F32)
                FG = 2
                if moe_layer:
                    experts = [(I["moe_w_gate"][e], I["moe_w_up"][e], I["moe_w_down"][e], EXP_DIM) for e in range(N_EXP)]
                else:
                    experts = [(I["ffn_w_gate"][l // 2], I["ffn_w_up"][l // 2], I["ffn_w_down"][l // 2], FFN_DIM)]
                wgr = P.ring("fwg", [128, 16, FG * 128], BF16, 2)
                wur = P.ring("fwu", [128, 16, FG * 128], BF16, 2)
                wdr = P.ring("fwd", [128, FG, D], BF16, 2)
                utr = P.ring("fut", [128, 4, 16, 128], BF16, 2)
                hsr = P.ring("fhs", [128, 512], F32, 2)
                hTr = P.ring("fhT", [128, FG, 512], BF16, 2)
                yacc = P.sb("yacc", [128, 4, D], F32)
                gxr = P.ring("fgx", [128, 4, 8], F32, 2)
                xmr = P.ring("fxm", [128, D], F32, 2)
                zr = P.ring("fz2", [128, D], F32, 2)
                str_ = P.ring("fst", [128, 4, 6], F32, 2)
                mvr = P.ring("fmv", [128, 2], F32, 2)
                rsr = P.ring("frs", [128, 1], F32, 2)
                nmr_ = P.ring("fnm", [128, 1], F32, 2)
                pgr = P.ring("pg", [128, 512], F32, 2, psum=True)
                pur = P.ring("pu", [128, 512], F32, 2, psum=True)
                pdr = P.ring("pdn", [128, 512], F32, 4, psum=True)
                supers = []
                if not last:
                    supers.append([0, 1])
                for s0 in range(2, NT, 4):
                    supers.append(list(range(s0, s0 + 4)))
                cur_s = None
                for sup in supers:
                    s = 1 if sup[0] < 2 else 0
                    if s != cur_s:
                        P.ld(gate2, gate2[:], bcast(S["mod"][l, s, 5 * D:6 * D]))
                        cur_s = s
                    nt_ = len(sup)
                    ntok = nt_ * 128
                    ut = utr.nxt()
                    for ti, j in enumerate(sup):
                        P.ld(ut, ut[:, ti, :, :], S["u2T"][j])
                    if moe_layer:
                        gx = gxr.nxt()
                        for ti, j in enumerate(sup):
                            P.ld(gx, gx[:, ti, :], S["gexp"][j * 128:(j + 1) * 128, :])
                    first_acc = True
                    for ei, (Wg, Wu, Wd, FD) in enumerate(experts):
                        for f0 in range(0, FD // 128, FG):
                            wg, wu, wd = wgr.nxt(), wur.nxt(), wdr.nxt()
                            fc = slice(f0 * 128, (f0 + FG) * 128)
                            P.ld(wg, wg[:], Wg[:, fc].rearrange("(c p) f -> p c f", p=128), q="pool")
                            P.ld(wu, wu[:], Wu[:, fc].rearrange("(c p) f -> p c f", p=128), q="pool")
                            P.ld(wd, wd[:], Wd[fc, :].rearrange("(b p) d -> p b d", p=128), q="pool")
                            hT = hTr.nxt()
                            for fb in range(FG):
                                pg, pu = pgr.nxt(), pur.nxt()
                                fs = slice(fb * 128, (fb + 1) * 128)
                                for c in range(16):
                                    P.mm(pg[:, 0:ntok].rearrange("p (t k) -> p t k", k=128), wg[:, c, fs],
                                         ut[:, 0:nt_, c, :], c == 0, c == 15, [wg, ut], [pg])
                                for c in range(16):
                                    P.mm(pu[:, 0:ntok].rearrange("p (t k) -> p t k", k=128), wu[:, c, fs],
                                         ut[:, 0:nt_, c, :], c == 0, c == 15, [wu, ut], [pu])
                                hs = hsr.nxt()
                                P.act(hs[:, 0:ntok], pg[:, 0:ntok], AF.Silu, [pg], [hs])
                                P.tt("dve", hT[:, fb, 0:ntok], hs[:, 0:ntok], pu[:, 0:ntok], ALU.mult, [hs, pu], [hT])
                            for ti in range(nt_):
                                for nb in range(4):
                                    nbs = slice(nb * 512, (nb + 1) * 512)
                                    pd = pdr.nxt()
                                    for fb in range(FG):
                                        P.mm(pd[:], hT[:, fb, ti * 128:(ti + 1) * 128], wd[:, fb, nbs], fb == 0,
                                             fb == FG - 1, [hT, wd], [pd])
                                    if moe_layer:
                                        if first_acc:
                                            P.ts("dve", yacc[:, ti, nbs], pd[:], gx[:, ti, ei:ei + 1], None, ALU.mult, None,
                                                 [pd, gx], [yacc])
                                        else:
                                            P.stt("dve", yacc[:, ti, nbs], pd[:], gx[:, ti, ei:ei + 1], yacc[:, ti, nbs],
                                                  ALU.mult, ALU.add, [pd, gx, yacc], [yacc])
                                    else:
                                        if first_acc:
                                            P.cp("dve", yacc[:, ti, nbs], pd[:], [pd], [yacc])
                                        else:
                                            P.tt("dve", yacc[:, ti, nbs], yacc[:, ti, nbs], pd[:], ALU.add, [yacc, pd], [yacc])
                            first_acc = False
                    for ti, j in enumerate(sup):
                        rows = slice(j * 128, (j + 1) * 128)
                        xm, z = xmr.nxt(), zr.nxt()
                        P.ld(xm, xm[:], S["xmid"][rows, :])
                        P.tt("dve", z[:], yacc[:, ti, :], gate2[:], ALU.mult, [yacc, gate2], [z])
                        P.stt("dve", z[:], xm[:], ALPHA, z[:], ALU.mult, ALU.add, [xm, z], [z])
                        st, mv, rstd, nmr = str_.nxt(), mvr.nxt(), rsr.nxt(), nmr_.nxt()
                        ln_stats(P, z, z, st, mv, rstd, nmr)
                        P.act(xm[:], z[:], AF.Identity, [z, rstd, nmr], [xm], bias=nmr[:], scale=rstd[:])
                        P.tt("dve", xm[:], xm[:], g2[:], ALU.mult, [xm, g2], [xm])
                        P.tt("pool", xm[:], xm[:], b2[:], ALU.add, [xm, b2], [xm])
                        if last:
                            P.st(xm, y_out[(j - 2) * 128:(j - 1) * 128, :], xm[:])
                        else:
                            P.st(xm, S["xcur"][rows, :], xm[:])
                P.end_phase()
            if stop_after == ("E", l):
                break
        P.begin(es)
        fin = P.sb("fin", [128, 8], F32)
        P.memset("pool", fin[:], 0.0, [fin])
        P.end_phase()
    return nc


_CACHE = {}


def prep_inputs(inputs, b, layers=DEPTH):
    f = lambda a: np.ascontiguousarray(np.asarray(a, dtype=np.float32))
    m = {}
    m["x"] = f(inputs["x"][b])
    m["ctx"] = f(inputs["ctx"][b])
    m["c"] = f(np.stack([inputs["c"][b], inputs["c_ctx"]], 0).reshape(2, 16, 128).transpose(2, 1, 0).reshape(128, 32))
    m["w_ada"] = f(inputs["w_ada"])
    m["b_ada"] = f(inputs["b_ada"])
    m["w_in"] = f(inputs["w_in"])
    m["conv_w"] = f(np.asarray(inputs["conv_w"]).reshape(DEPTH, 9, D))
    m["conv_b"] = f(inputs["conv_b"])
    m["mlstm_gate_b"] = f(inputs["mlstm_gate_b"])
    m["hgrn_lb"] = f(inputs["hgrn_lb"])
    m["ret_decay_logit"] = f(np.asarray(inputs["ret_decay_logit"]).reshape(DEPTH, 8))
    m["head_norm_g"] = f(np.asarray(inputs["head_norm_g"]).reshape(DEPTH, 3 * BW))
    m["w_branch"] = f(np.asarray(inputs["w_branch"]).reshape(DEPTH, 3 * BW, D))
    m["w_out"] = f(inputs["w_out"])
    m["post_ln_g"] = f(inputs["post_ln_g"])
    m["post_ln_b"] = f(inputs["post_ln_b"])
    m["ffn_w_gate"] = f(inputs["ffn_w_gate"])
    m["ffn_w_up"] = f(inputs["ffn_w_up"])
    m["ffn_w_down"] = f(inputs["ffn_w_down"])
    if layers > 1:
        m["moe_w_router"] = f(np.asarray(inputs["moe_w_router"])[0].T)
        m["moe_w_gate"] = f(np.asarray(inputs["moe_w_gate"])[0])
        m["moe_w_up"] = f(np.asarray(inputs["moe_w_up"])[0])
        m["moe_w_down"] = f(np.asarray(inputs["moe_w_down"])[0])
    m["consts"] = make_consts()
    return m


def kernel(**inputs):
    if "nc" not in _CACHE:
        _CACHE["nc"] = build_program()
    nc = _CACHE["nc"]
    names = list(build_program.in_names.keys())
    in_maps = [{k: v for k, v in prep_inputs(inputs, b).items() if k in names} for b in range(8)]
    res = run_bass_kernel_spmd(nc, in_maps, core_ids=list(range(8)))
    return np.stack([np.asarray(r["y"], dtype=np.float32) for r in res.results], 0)
```

```python
import os
from contextlib import ExitStack
import numpy as np
import concourse.bass as bass
import concourse.mybir as mybir
from concourse.bass_utils import run_bass_kernel_spmd

F32 = mybir.dt.float32
BF16 = mybir.dt.bfloat16
AF = mybir.ActivationFunctionType
ALU = mybir.AluOpType

D = 2048
DEPTH = 2
SEQ = 4096
CTX = 256
T = SEQ + CTX
NT = T // 128
NCH = T // 64
BW = 1024
N_IN = 18448
FFN_DIM = 5632
EXP_DIM = 7168
N_EXP = 8
ALPHA = (2 * DEPTH) ** 0.25
EPS = 1e-6
TINY = 1e-30
RFW, RBW = 31, 32

G_MQK, G_MV, G_MZ, G_GATE, G_HQ, G_HI, G_HFF, G_HFB, G_HG, G_RQ, G_RK, G_RV, G_RG, G_MERGE = range(14)
IN_SIZES = (2048, 1024, 1024, 16, 1024, 1024, 1024, 1024, 1024, 512, 512, 1024, 1024, 6144)
IN_OFF = [0]
for _s in IN_SIZES:
    IN_OFF.append(IN_OFF[-1] + _s)

ENGS = ("pe", "act", "dve", "pool", "sp")
HMAP = {"pe": "tensor", "act": "scalar", "dve": "vector", "pool": "gpsimd", "sp": "sync"}


class Buf:
    __slots__ = ("w", "r")

    def __init__(self):
        self.w = None
        self.r = []


class TL:
    def __init__(self, t):
        self.t = t
        self.b = Buf()
        self.slot = None

    def __getitem__(self, k):
        return self.t[k]


class Ring:
    def __init__(self, tiles):
        self.tiles = tiles
        self.i = 0

    def nxt(self):
        t = self.tiles[self.i % len(self.tiles)]
        self.i += 1
        return t


class Prog:
    def __init__(self, nc, es, n_dma=88):
        self.nc = nc
        self.ops = {e: [] for e in ENGS}
        self.cnt = {e: 0 for e in ENGS}
        self.sem = {e: es.enter_context(nc.semaphore("c_" + e)) for e in ("pe", "act", "dve", "pool")}
        self.slots = [[es.enter_context(nc.semaphore("d%d" % i)), 0] for i in range(n_dma)]
        self.slot_i = 0
        self.waited = {e: {} for e in ENGS}
        self.pes = None
        self.skip = ()
        self.mute = False
        self.pending = []
        self.pending_src = set()

    def begin(self, pes, name=None):
        self.pes = pes
        self.slot_i = 0
        self.mute = name in self.skip

    def slot(self):
        s = self.slots[self.slot_i]
        self.slot_i += 1
        return s

    def sb(self, name, shape, dt):
        self.uid = getattr(self, "uid", 0) + 1
        return TL(self.pes.enter_context(self.nc.sbuf_tensor("%s_%d" % (name, self.uid), list(shape), dt)))

    def ps(self, name, shape, dt):
        self.uid = getattr(self, "uid", 0) + 1
        return TL(self.pes.enter_context(self.nc.psum_tensor("%s_%d" % (name, self.uid), list(shape), dt)))

    def ring(self, name, shape, dt, n, psum=False):
        mk = self.ps if psum else self.sb
        return Ring([mk("%s%d" % (name, i), shape, dt) for i in range(n)])

    def _waits(self, eng, reads, writes):
        need = {}
        toks = []
        for t in reads:
            if t.b.w is not None:
                toks.append(t.b.w)
        for t in writes:
            if t.b.w is not None:
                toks.append(t.b.w)
            toks.extend(t.b.r)
        for tk in toks:
            if tk[0] == "c":
                if tk[1] == eng and eng == "pe":
                    continue
                key = tk[1]
                if need.get(key, (None, 0))[1] < tk[2]:
                    need[key] = (self.sem[tk[1]], tk[2])
            else:
                key = id(tk[1])
                if need.get(key, (None, 0))[1] < tk[2]:
                    need[key] = (tk[1][0], tk[2])
        out = []
        wd = self.waited[eng]
        for key, (s, v) in need.items():
            if wd.get(key, 0) >= v:
                continue
            wd[key] = v
            out.append((s, v))
        return out

    def _commit(self, tok, reads, writes):
        for t in reads:
            t.b.r.append(tok)
        for t in writes:
            t.b.w = tok
            t.b.r = []

    def op(self, eng, fn, reads=(), writes=()):
        if self.mute:
            return
        self._maybe_flush(writes)
        waits = self._waits(eng, reads, writes)
        self.cnt[eng] += 1
        tok = ("c", eng, self.cnt[eng])
        self.ops[eng].append((waits, fn, (self.sem[eng], 1)))
        self._commit(tok, reads, writes)

    def dma(self, q, out, in_, tile, reads=(), writes=(), slow=False):
        if self.mute:
            return
        self._maybe_flush(writes)
        if tile.slot is None or tile.slot[2] != id(self.pes):
            s = self.slot()
            tile.slot = (s[0], s, id(self.pes))
        s = tile.slot[1]
        waits = self._waits(q, reads, writes)
        s[1] += 16
        tok = ("d", s, s[1])
        if slow:
            self.ops[q].append((waits, lambda h: h.dma_start(out=out, in_=in_, allow_slow_non_contiguous=True),
                                (s[0], 16)))
        else:
            self.ops[q].append((waits, lambda h: h.dma_start(out=out, in_=in_), (s[0], 16)))
        self._commit(tok, reads, writes)

    def ld(self, tile, out, in_, q="sp", slow=False):
        self.dma(q, out, in_, tile, writes=[tile], slow=slow)

    def st(self, tile, out, in_, q="sp"):
        if self.mute:
            return
        self.pending.append((tile, out, in_, q))
        self.pending_src.add(id(tile))

    def _flush(self):
        pend, self.pending, self.pending_src = self.pending, [], set()
        for tile, out, in_, q in pend:
            self.dma(q, out, in_, tile, reads=[tile])

    def _maybe_flush(self, writes):
        if self.pending:
            for t in writes:
                if id(t) in self.pending_src:
                    self._flush()
                    return

    def end_phase(self):
        self._flush()
        nc = self.nc
        allw = [(self.sem[e], self.cnt[e], e) for e in ("pe", "act", "dve", "pool") if self.cnt[e] > 0]
        alld = [(s[0], s[1], id(s)) for s in self.slots if s[1] > 0]
        for e in ENGS:
            wd = self.waited[e]
            ws = []
            for s, v, key in allw + alld:
                if wd.get(key, 0) >= v:
                    continue
                wd[key] = v
                ws.append((s, v))
            self.ops[e].append((ws, None, None))
        with nc.Block() as block:
            for e in ENGS:
                lst = self.ops[e]

                def body(h, lst=lst):
                    for waits, fn, inc in lst:
                        for s, v in waits:
                            h.wait_ge(s, v)
                        if fn is not None:
                            fn(h).then_inc(inc[0], inc[1])
                getattr(block, HMAP[e])(body)
        self.ops = {e: [] for e in ENGS}

    def mm(self, out, lhsT, rhs, start, stop, reads, writes):
        self.op("pe", lambda h: h.matmul(out, lhsT=lhsT, rhs=rhs, start=start, stop=stop), reads, writes)

    def tr(self, out, in_, ident, reads, writes):
        self.op("pe", lambda h: h.transpose(out, in_, ident), reads, writes)

    def act(self, out, in_, func, reads, writes, bias=None, scale=None):
        kw = {}
        if bias is not None:
            kw["bias"] = bias
        if scale is not None:
            kw["scale"] = scale
        self.op("act", lambda h: h.activation(out=out, in_=in_, func=func, **kw), reads, writes)

    def tt(self, eng, out, in0, in1, op, reads, writes):
        self.op(eng, lambda h: h.tensor_tensor(out=out, in0=in0, in1=in1, op=op), reads, writes)

    def ts(self, eng, out, in0, s1, s2, op0, op1, reads, writes, accum_out=None):
        if accum_out is not None:
            self.op(eng, lambda h: h.tensor_scalar(out=out, in0=in0, scalar1=s1, scalar2=s2, op0=op0, op1=op1,
                                                   accum_out=accum_out), reads, writes)
        elif op1 is None:
            self.op(eng, lambda h: h.tensor_scalar(out=out, in0=in0, scalar1=s1, scalar2=None, op0=op0), reads, writes)
        else:
            self.op(eng, lambda h: h.tensor_scalar(out=out, in0=in0, scalar1=s1, scalar2=s2, op0=op0, op1=op1),
                    reads, writes)

    def stt(self, eng, out, in0, scalar, in1, op0, op1, reads, writes):
        self.op(eng, lambda h: h.scalar_tensor_tensor(out=out, in0=in0, scalar=scalar, in1=in1, op0=op0, op1=op1),
                reads, writes)

    def cp(self, eng, out, in_, reads, writes):
        if eng == "act":
            self.op("act", lambda h: h.activation(out=out, in_=in_, func=AF.Copy), reads, writes)
        else:
            self.op(eng, lambda h: h.tensor_copy(out=out, in_=in_), reads, writes)

    def memset(self, eng, ap, val, writes):
        self.op(eng, lambda h: h.memset(ap, val), (), writes)


C_IDENT = 0
C_MTF = 128
C_MTB = 192
C_CSF = 256
C_CSB = 320
C_INDF = 384
C_INDB = 386
C_INDBF_L = 388
C_INDBF_M = 516
C_INDBB_L = 644
C_INDBB_M = 772
C_ML = 900
C_MR = 901
C_ONE = 902
C_N = 904


def make_consts():
    c = np.zeros((128, C_N), np.float32)
    c[:, C_IDENT:C_IDENT + 128] = np.eye(128, dtype=np.float32)
    s = np.arange(64)
    c[:64, C_MTF:C_MTF + 64] = (s[:, None] <= s[None, :])
    c[:64, C_MTB:C_MTB + 64] = (s[:, None] >= s[None, :])
    c[:64, C_CSF:C_CSF + 64] = (s[:, None] <= s[None, :]).astype(np.float32) - (s[:, None] <= RFW)
    c[:64, C_CSB:C_CSB + 64] = (s[:, None] >= s[None, :]).astype(np.float32) - (s[:, None] >= RBW)
    c[:64, C_INDF] = s > RFW
    c[:64, C_INDF + 1] = s <= RFW
    c[:64, C_INDB] = s < RBW
    c[:64, C_INDB + 1] = s >= RBW
    c[:64, C_INDBF_L:C_INDBF_L + 128] = (s > RFW)[:, None]
    c[:64, C_INDBF_M:C_INDBF_M + 128] = (s <= RFW)[:, None]
    c[:64, C_INDBB_L:C_INDBB_L + 128] = (s < RBW)[:, None]
    c[:64, C_INDBB_M:C_INDBB_M + 128] = (s >= RBW)[:, None]
    p = np.arange(128)
    c[:, C_ML] = (p % 64) != 0
    c[:, C_MR] = (p % 64) != 63
    c[:, C_ONE] = 1.0
    return c


def build_program(dbg=None, layers=DEPTH, stop_after=None, skip=(), scan_stage=9, scan_nch=None):
    nc = bass.Bass("TRN2", target_bir_lowering=False)
    dbg = dbg or []

    def din(name, shape):
        return nc.dram_tensor(name, list(shape), F32, kind="ExternalInput").ap()

    def dsc(name, shape, dt):
        kind = "ExternalOutput" if name in dbg else "Internal"
        return nc.dram_tensor(name, list(shape), dt, kind=kind).ap()

    specs = dict(
        x=[SEQ, D], c=[128, 32], ctx=[CTX, D], w_ada=[DEPTH, D, 6 * D], b_ada=[DEPTH, 6 * D],
        w_in=[DEPTH, D, N_IN], conv_w=[DEPTH, 9, D], conv_b=[DEPTH, D], mlstm_gate_b=[DEPTH, 16],
        hgrn_lb=[2, DEPTH, BW], ret_decay_logit=[DEPTH, 8], head_norm_g=[DEPTH, 3 * BW],
        w_branch=[DEPTH, 3 * BW, D], w_out=[DEPTH, D, D], post_ln_g=[DEPTH, 2, D], post_ln_b=[DEPTH, 2, D],
        ffn_w_gate=[1, D, FFN_DIM], ffn_w_up=[1, D, FFN_DIM], ffn_w_down=[1, FFN_DIM, D],
        moe_w_router=[N_EXP, D], moe_w_gate=[N_EXP, D, EXP_DIM], moe_w_up=[N_EXP, D, EXP_DIM],
        moe_w_down=[N_EXP, EXP_DIM, D], consts=[128, C_N])

    class Lazy(dict):
        def __missing__(self, k):
            self[k] = din(k, specs[k])
            return self[k]
    I = Lazy()
    build_program.in_names = I
    y_out = nc.dram_tensor("y", [SEQ, D], F32, kind="ExternalOutput").ap()

    S = dict(
        mod=dsc("mod", [DEPTH, 2, 6 * D], F32),
        uT=dsc("uT", [NT, 128, 16, 128], BF16),
        mqk=dsc("mqk", [T + 384, D], BF16),
        qk=dsc("qk", [T, D], BF16), mv=dsc("mv", [T, BW], BF16), zs=dsc("zs", [T, 3 * BW], BF16),
        gt=dsc("gt", [T, 16], F32), hq=dsc("hq", [T, BW], BF16), hi=dsc("hi", [T, BW], BF16),
        kk=dsc("kk", [2, T, BW], BF16), lf=dsc("lf", [2, T, BW], F32),
        rqk=dsc("rqk", [T, BW], BF16), rv=dsc("rv", [T, BW], BF16), mg=dsc("mg", [T, 3 * D], BF16),
        bxT=dsc("bxT", [NT, 128, 24, 128], BF16),
        mixT=dsc("mixT", [NT, 128, 16, 128], BF16),
        xmid=dsc("xmid", [T, D], F32), u2T=dsc("u2T", [NT, 128, 16, 128], BF16),
        gexp=dsc("gexp", [T, N_EXP], F32), xcur=dsc("xcur", [T, D], F32),
        ysc=dsc("ysc", [2, T, 3 * BW], F32), u2f=dsc("u2f", [T, D], F32),
    )

    def mqk_row(tok):
        return tok + 128 if tok < CTX else tok + 256

    with ExitStack() as es:
        P = Prog(nc, es)
        P.skip = tuple(skip)

        def xsrc(l, j):
            if l == 0:
                return I["ctx"][j * 128:(j + 1) * 128, :] if j < 2 else I["x"][(j - 2) * 128:(j - 1) * 128, :]
            return S["xcur"][j * 128:(j + 1) * 128, :]

        def bcast(ap_row):
            return ap_row.partition_broadcast(128)

        def load_ident(P):
            cf = P.sb("idf", [128, 128], F32)
            ib = P.sb("idb", [128, 128], BF16)
            P.ld(cf, cf[:], I["consts"][:, C_IDENT:C_IDENT + 128])
            P.cp("dve", ib[:], cf[:], [cf], [ib])
            return ib

        def ln_stats(P, x_ap, xt, st, mv, rstd, nmr, n=D):
            nchunk = n // 512
            for k in range(nchunk):
                P.op("dve", lambda h, k=k: h.bn_stats(out=st[:, k, :], in_=x_ap[:, k * 512:(k + 1) * 512]),
                     [xt], [st])
            P.op("dve", lambda h: h.bn_aggr(out=mv[:], in_=st[:, 0:nchunk, :]), [st], [mv])
            P.ts("dve", rstd[:], mv[:, 1:2], EPS, None, ALU.add, None, [mv], [rstd])
            P.act(rstd[:], rstd[:], AF.Sqrt, [rstd], [rstd])
            P.op("dve", lambda h: h.reciprocal(out=rstd[:], in_=rstd[:]), [rstd], [rstd])
            P.stt("dve", nmr[:], mv[:, 0:1], -1.0, rstd[:], ALU.mult, ALU.mult, [mv, rstd], [nmr])

        def transpose_to(P, src, src_t, nchunks, ident, ptr, dst, evac_engs=("act", "dve")):
            for g in range(0, nchunks, 8):
                pt = ptr.nxt()
                n = min(8, nchunks - g)
                for k in range(n):
                    P.tr(pt[:, k, :], src[:, (g + k) * 128:(g + k + 1) * 128], ident[:], [src_t, ident], [pt])
                P.cp(evac_engs[(g // 8) % len(evac_engs)], dst[:, g:g + n, :], pt[:, 0:n, :], [pt], [dst])

        with ExitStack() as pes:
            P.begin(pes, "0")
            cT = P.sb("cT", [128, 16, 2], F32)
            P.ld(cT, cT[:], I["c"].rearrange("p (k s) -> p k s", s=2))
            P.act(cT[:], cT[:], AF.Silu, [cT], [cT])
            wr = P.ring("wada", [128, 16, 512], F32, 2)
            pr = P.ring("pada", [2, 512], F32, 2, psum=True)
            orr = P.ring("oada", [2, 512], F32, 2)
            bada = P.sb("bada", [2, 512], F32)
            for l in range(layers):
                for nb in range(24):
                    w = wr.nxt()
                    P.ld(w, w[:], I["w_ada"][l][:, nb * 512:(nb + 1) * 512].rearrange("(k p) n -> p k n", p=128))
                    P.ld(bada, bada[:], I["b_ada"][l:l + 1, nb * 512:(nb + 1) * 512].broadcast_to([2, 512]))
                    pp = pr.nxt()
                    for k in range(16):
                        P.mm(pp[:], cT[:, k, :], w[:, k, :], k == 0, k == 15, [cT, w], [pp])
                    o = orr.nxt()
                    P.tt("dve", o[:], pp[:], bada[:], ALU.add, [pp, bada], [o])
                    P.st(o, S["mod"][l][:, nb * 512:(nb + 1) * 512], o[:])
            P.end_phase()

        for l in range(layers):
            last = (l == DEPTH - 1)
            with ExitStack() as pes:
                P.begin(pes, "A")
                ident = load_ident(P)
                vec = {}
                for s in range(2):
                    sh = P.sb("sh%d" % s, [128, D], F32)
                    sc = P.sb("sc%d" % s, [128, D], F32)
                    P.ld(sh, sh[:], bcast(S["mod"][l, s, 0:D]))
                    P.ld(sc, sc[:], bcast(S["mod"][l, s, D:2 * D]))
                    P.ts("pool", sc[:], sc[:], 1.0, None, ALU.add, None, [sc], [sc])
                    vec[s] = (sh, sc)
                xr = P.ring("xa", [128, D], F32, 3)
                xnr = P.ring("xn", [128, D], F32, 2)
                ur = P.ring("ua", [128, D], BF16, 2)
                utr = P.ring("uta", [128, 16, 128], BF16, 2)
                ptr = P.ring("pta", [128, 8, 128], BF16, 4, psum=True)
                str_ = P.ring("sta", [128, 4, 6], F32, 2)
                mvr = P.ring("mva", [128, 2], F32, 2)
                rsr = P.ring("rsa", [128, 1], F32, 2)
                nmr_ = P.ring("nma", [128, 1], F32, 2)
                for j in range(NT):
                    s = 1 if j < 2 else 0
                    sh, sc = vec[s]
                    xt = xr.nxt()
                    P.ld(xt, xt[:], xsrc(l, j))
                    st, mv, rstd, nmr = str_.nxt(), mvr.nxt(), rsr.nxt(), nmr_.nxt()
                    ln_stats(P, xt, xt, st, mv, rstd, nmr)
                    xn = xnr.nxt()
                    P.act(xn[:], xt[:], AF.Identity, [xt, rstd, nmr], [xn], bias=nmr[:], scale=rstd[:])
                    P.tt("dve", xn[:], xn[:], sc[:], ALU.mult, [xn, sc], [xn])
                    u = ur.nxt()
                    P.tt("pool", u[:], xn[:], sh[:], ALU.add, [xn, sh], [u])
                    ut = utr.nxt()
                    transpose_to(P, u, u, 16, ident, ptr, ut)
                    P.st(ut, S["uT"][j], ut[:])
                P.end_phase()
            if stop_after == ("A", l):
                break

            with ExitStack() as pes:
                P.begin(pes, "B")
                gb = P.sb("gb", [128, 16], F32)
                P.ld(gb, gb[:], bcast(I["mlstm_gate_b"][l]))
                lbt, omlt = [], []
                for d in range(2):
                    lb = P.sb("lb%d" % d, [128, BW], F32)
                    oml = P.sb("oml%d" % d, [128, BW], F32)
                    if l == 0:
                        P.memset("pool", lb[:], 0.0, [lb])
                    else:
                        P.ld(lb, lb[:], bcast(I["hgrn_lb"][d, 1]))
                        P.ld(oml, oml[:], bcast(I["hgrn_lb"][d, 0]))
                        P.tt("dve", lb[:], lb[:], oml[:], ALU.subtract, [lb, oml], [lb])
                        P.act(lb[:], lb[:], AF.Sigmoid, [lb], [lb])
                    P.ts("dve", oml[:], lb[:], -1.0, 1.0, ALU.mult, ALU.add, [lb], [oml])
                    lbt.append(lb)
                    omlt.append(oml)
                zt = P.sb("zt", [128, D], BF16)
                P.memset("pool", zt[:], 0.0, [zt])
                for r0 in (0, 128 + CTX, 256 + T):
                    P.st(zt, S["mqk"][r0:r0 + 128, :], zt[:])
                wr = P.ring("wb", [128, 16, 1024], BF16, 2)
                utr = P.ring("utb", [128, 16, 128], BF16, 3)
                pr = P.ring("pb", [128, 512], F32, 6, psum=True)
                o16 = P.ring("ob", [128, 512], BF16, 8)
                o32 = P.ring("of", [128, 512], F32, 4)
                t32 = P.ring("tf", [128, 512], F32, 3)
                g32 = P.ring("gf", [128, 16], F32, 2)
                blocks = []
                for g in range(14):
                    for c0 in range(0, IN_SIZES[g], 1024):
                        blocks.append((g, c0, min(1024, IN_SIZES[g] - c0)))
                ei = [0]

                def epilogue(g, pp, j, c0, n):
                    rows = slice(j * 128, (j + 1) * 128)
                    cs = slice(c0, c0 + n)
                    ei[0] += 1

                    def simple(dst_ap, func=None, scale=None):
                        o = o16.nxt()
                        if func is None and scale is None and ei[0] % 2 == 0:
                            P.cp("dve", o[:, 0:n], pp[:, 0:n], [pp], [o])
                        else:
                            P.act(o[:, 0:n], pp[:, 0:n], func or AF.Copy, [pp], [o], scale=scale)
                        P.st(o, dst_ap, o[:, 0:n])
                    if g == G_MQK:
                        r0 = mqk_row(j * 128)
                        simple(S["mqk"][r0:r0 + 128, cs])
                    elif g == G_MV:
                        simple(S["mv"][rows, cs])
                    elif g == G_MZ:
                        simple(S["zs"][rows, c0:c0 + n], AF.Silu)
                    elif g == G_HG:
                        simple(S["zs"][rows, BW + c0:BW + c0 + n], AF.Silu)
                    elif g == G_RG:
                        simple(S["zs"][rows, 2 * BW + c0:2 * BW + c0 + n], AF.Silu)
                    elif g == G_HQ:
                        simple(S["hq"][rows, cs], AF.Silu)
                    elif g == G_HI:
                        simple(S["hi"][rows, cs])
                    elif g == G_RQ:
                        simple(S["rqk"][rows, 0:512])
                    elif g == G_RK:
                        simple(S["rqk"][rows, 512:1024], None, 128.0 ** -0.5)
                    elif g == G_RV:
                        simple(S["rv"][rows, cs])
                    elif g == G_MERGE:
                        simple(S["mg"][rows, cs], AF.Sigmoid)
                    elif g == G_GATE:
                        o = g32.nxt()
                        P.tt("dve", o[:], pp[:, 0:16], gb[:], ALU.add, [pp, gb], [o])
                        for q0 in (4, 12):
                            P.act(o[:, q0:q0 + 4], o[:, q0:q0 + 4], AF.Sigmoid, [o], [o])
                            P.act(o[:, q0:q0 + 4], o[:, q0:q0 + 4], AF.Ln, [o], [o])
                        P.st(o, S["gt"][rows, :], o[:])
                    else:
                        d = 0 if g == G_HFF else 1
                        tmp = t32.nxt()
                        P.act(tmp[:, 0:n], pp[:, 0:n], AF.Sigmoid, [pp], [tmp])
                        P.tt("dve", tmp[:, 0:n], tmp[:, 0:n], omlt[d][:, cs], ALU.mult, [tmp, omlt[d]], [tmp])
                        P.stt("dve", tmp[:, 0:n], tmp[:, 0:n], TINY, lbt[d][:, cs], ALU.max, ALU.add,
                              [tmp, lbt[d]], [tmp])
                        o = o32.nxt()
                        P.act(o[:, 0:n], tmp[:, 0:n], AF.Ln, [tmp], [o])
                        P.st(o, S["lf"][d, rows, cs], o[:, 0:n])
                        ob = o16.nxt()
                        P.ts("pool", ob[:, 0:n], tmp[:, 0:n], -1.0, 1.0, ALU.mult, ALU.add, [tmp], [ob])
                        P.st(ob, S["kk"][d, rows, cs], ob[:, 0:n])

                for (g, c0, n) in blocks:
                    w = wr.nxt()
                    col = IN_OFF[g] + c0
                    P.ld(w, w[:, :, 0:n], I["w_in"][l][:, col:col + n].rearrange("(k p) n -> p k n", p=128), q="pool")
                    for j in range(NT):
                        ut = utr.nxt()
                        P.ld(ut, ut[:], S["uT"][j])
                        for h0 in range(0, n, 512):
                            hn = min(512, n - h0)
                            pp = pr.nxt()
                            for k in range(16):
                                P.mm(pp[:, 0:hn], ut[:, k, :], w[:, k, h0:h0 + hn], k == 0, k == 15, [ut, w], [pp])
                            epilogue(g, pp, j, c0 + h0, hn)
                P.end_phase()
            if stop_after == ("B", l):
                break

            tiles_post = list(range(2, NT)) if last else list(range(NT))

            with ExitStack() as pes:
                P.begin(pes, "C")
                cm = P.sb("cm", [128, 2], F32)
                P.ld(cm, cm[:], I["consts"][:, C_ML:C_ML + 2])
                cb = P.sb("cb", [128, D], F32)
                P.ld(cb, cb[:], bcast(I["conv_b"][l]))
                wts = []
                for tap in range(9):
                    w = P.sb("wm%d" % tap, [128, D], F32)
                    P.ld(w, w[:], bcast(I["conv_w"][l, tap]))
                    wts.append(w)
                xsr = P.ring("xs", [128, D], BF16, 4)
                accr = P.ring("acc", [128, D], F32, 2)
                tmpr = P.ring("ctmp", [128, D], F32, 2)
                outr = P.ring("cout", [128, D], BF16, 2)

                def conv_tile(j, taps):
                    acc = accr.nxt()
                    r0 = mqk_row(j * 128)
                    for i, (off, w) in enumerate(taps):
                        xs = xsr.nxt()
                        P.ld(xs, xs[:], S["mqk"][r0 + off:r0 + off + 128, :])
                        if i == 0:
                            P.tt("pool", acc[:], xs[:], w[:], ALU.mult, [xs, w], [acc])
                        else:
                            tmp = tmpr.nxt()
                            P.tt("pool", tmp[:], xs[:], w[:], ALU.mult, [xs, w], [tmp])
                            P.tt("dve", acc[:], acc[:], tmp[:], ALU.add, [acc, tmp], [acc])
                    P.tt("dve", acc[:], acc[:], cb[:], ALU.add, [acc, cb], [acc])
                    o = outr.nxt()
                    P.act(o[:], acc[:], AF.Silu, [acc], [o])
                    P.ts("dve", o[:, BW:2 * BW], o[:, BW:2 * BW], 0.0625, None, ALU.mult, None, [o], [o])
                    P.st(o, S["qk"][j * 128:(j + 1) * 128, :], o[:])

                for j in range(2):
                    conv_tile(j, [(-1, wts[3]), (0, wts[4]), (1, wts[5])])
                for tap in (0, 3, 6):
                    P.ts("dve", wts[tap][:], wts[tap][:], cm[:, 0:1], None, ALU.mult, None, [wts[tap], cm], [wts[tap]])
                for tap in (2, 5, 8):
                    P.ts("dve", wts[tap][:], wts[tap][:], cm[:, 1:2], None, ALU.mult, None, [wts[tap], cm], [wts[tap]])
                for j in range(2, NT):
                    conv_tile(j, [((tap // 3 - 1) * 64 + (tap % 3 - 1), wts[tap]) for tap in range(9)])
                P.end_phase()
            if stop_after == ("C", l):
                break

            def scan_pass(d):
                with ExitStack() as pes:
                    P.begin(pes, "S%d" % d)
                    ident = load_ident(P)
                    cst = P.sb("cst", [64, 772], F32)
                    P.ld(cst, cst[:], I["consts"][0:64, 128:900])

                    def cc(c0, n):
                        return cst[:, c0 - 128:c0 - 128 + n]
                    maskT = cc(C_MTF if d == 0 else C_MTB, 64)
                    CS = cc(C_CSF if d == 0 else C_CSB, 64)
                    IND = cc(C_INDF if d == 0 else C_INDB, 2)
                    INDL = cc(C_INDBF_L if d == 0 else C_INDBB_L, 128)
                    INDM = cc(C_INDBF_M if d == 0 else C_INDBB_M, 128)
                    lg = P.sb("lg", [64, 4], F32)
                    P.ld(lg, lg[:], I["ret_decay_logit"][l, d * 4:(d + 1) * 4].partition_broadcast(64))
                    P.act(lg[:], lg[:], AF.Sigmoid, [lg], [lg])
                    P.act(lg[:], lg[:], AF.Ln, [lg], [lg])
                    lfsr = P.ring("lfs", [64, 8], F32, 2)
                    for t_ in lfsr.tiles:
                        P.cp("dve", t_[:, 4:8], lg[:], [lg], [t_])
                    Sm = P.sb("Sm", [128, 4, 514], F32)
                    Smb = P.sb("Smb", [128, 4, 514], BF16)
                    Sh = P.sb("Sh", [128, 8, 128], F32)
                    Shb = P.sb("Shb", [128, 8, 128], BF16)
                    Sr = P.sb("Sr", [128, 4, 256], F32)
                    Srb = P.sb("Srb", [128, 4, 256], BF16)
                    elp = P.sb("elp", [128, 16], F32)
                    for t_ in (Sm, Smb, Sh, Shb, Sr, Srb, elp):
                        P.memset("pool", t_[:], 0.0, [t_])
                    qkr = P.ring("sqk", [64, D], BF16, 2)
                    vmr = P.ring("svm", [64, 4, 257], BF16, 2)
                    for t_ in vmr.tiles:
                        P.memset("pool", t_[:, :, 256:257], 1.0, [t_])
                    gtr = P.ring("sgt", [64, 16], F32, 2)
                    hqr = P.ring("shq", [64, BW], BF16, 2)
                    hir = P.ring("shi", [64, BW], BF16, 2)
                    kkr = P.ring("skk", [64, BW], BF16, 2)
                    lfr = P.ring("slf", [64, BW], F32, 2)
                    rqr = P.ring("srq", [64, BW], BF16, 2)
                    rvr = P.ring("srv", [64, BW], BF16, 2)
                    eqr = P.ring("eqs", [64, 8], F32, 2)
                    ekr = P.ring("eks", [64, 8], F32, 2)
                    exr = P.ring("exs", [128, 16], F32, 2)
                    g16r = P.ring("g16", [128, 16], F32, 2)
                    e1r = P.ring("e1", [64, BW], F32, 2)
                    e2r = P.ring("e2", [64, BW], F32, 2)
                    qar = P.ring("QA", [64, 2560], BF16, 2)
                    kar = P.ring("KA", [64, 2560], BF16, 2)
                    qtr = P.ring("QT", [128, 20, 64], BF16, 2)
                    ktr = P.ring("KT", [128, 20, 64], BF16, 2)
                    ptr_ = P.ring("pT", [64, 16, 64], BF16, 2)
                    for t_ in ptr_.tiles:
                        P.memset("pool", t_[:], 0.0, [t_])
                    mfull = P.sb("mfull", [64, 8, 64], F32)
                    P.cp("dve", mfull[:], maskT.unsqueeze(1).to_broadcast([64, 8, 64]), [cst], [mfull])
                    yr_ = P.ring("Y", [64, 3 * BW], F32, 2)
                    dnr = P.ring("dn", [64, 1], F32, 4)
                    tpr = P.ring("tps", [128, 16, 64], BF16, 2, psum=True)
                    atr = P.ring("ats", [64, 8, 64], F32, 2, psum=True)
                    outr = P.ring("outs", [128, 512], F32, 2, psum=True)
                    dsr = P.ring("dss", [128, 512], F32, 1, psum=True)
                    gpr = P.ring("gps", [128, 64], F32, 1, psum=True)
                    units = [[2 * h, 2 * h + 1] for h in range(4)] + [[8 + h] for h in range(8)] + \
                            [[16 + h] for h in range(4)]
                    if d == 0:
                        order = list(range(NCH))
                    else:
                        order = [3, 2, 1, 0] + list(range(NCH - 1, 3, -1))
                    b3 = lambda ap, h, n: ap.unsqueeze(2).to_broadcast([ap.shape[0], h, n])
                    if scan_nch:
                        order = order[:scan_nch]
                    for ci in order:
                        r = slice(ci * 64, ci * 64 + 64)
                        qk, vm, gt, hq, hi = qkr.nxt(), vmr.nxt(), gtr.nxt(), hqr.nxt(), hir.nxt()
                        kk, lf, rq, rv = kkr.nxt(), lfr.nxt(), rqr.nxt(), rvr.nxt()
                        P.ld(gt, gt[:], S["gt"][r, :])
                        P.ld(lf, lf[:], S["lf"][d, r, :])
                        P.ld(qk, qk[:], S["qk"][r, :])
                        P.ld(hq, hq[:], S["hq"][r, :])
                        P.ld(kk, kk[:], S["kk"][d, r, :])
                        P.ld(rq, rq[:], S["rqk"][r, :])
                        P.ld(vm, vm[:, :, 0:256], S["mv"][r, :].rearrange("p (h c) -> p h c", h=4))
                        P.ld(hi, hi[:], S["hi"][r, :])
                        P.ld(rv, rv[:], S["rv"][r, :])
                        lfs = lfsr.nxt()
                        P.cp("dve", lfs[:, 0:4], gt[:, 8 * d + 4:8 * d + 8], [gt], [lfs])
                        gp = gpr.nxt()
                        P.mm(gp[0:64, 32:40], CS, lfs[:], True, True, [cst, lfs], [gp])
                        P.mm(gp[:, 0:8], INDL, lfs[:], True, True, [cst, lfs], [gp])
                        P.mm(gp[:, 8:16], INDM, lfs[:], True, True, [cst, lfs], [gp])
                        for h in range(8):
                            P.mm(gp[:, 16 + 2 * h:18 + 2 * h], lf[:, h * 128:(h + 1) * 128], IND, True, True,
                                 [lf, cst], [gp])
                        eqs, eks, ex, g16 = eqr.nxt(), ekr.nxt(), exr.nxt(), g16r.nxt()
                        P.act(eqs[:], gp[0:64, 32:40], AF.Exp, [gp], [eqs])
                        P.tt("dve", eks[:, 0:4], gt[:, 8 * d:8 * d + 4], gp[0:64, 32:36], ALU.subtract, [gt, gp], [eks])
                        P.act(eks[:, 0:4], eks[:, 0:4], AF.Exp, [eks], [eks])
                        P.act(eks[:, 4:8], gp[0:64, 36:40], AF.Exp, [gp], [eks], scale=-1.0)
                        gph = gp[:, 16:32].rearrange("p (h t) -> p h t", t=2)
                        P.tt("dve", ex[:, 0:4], gp[:, 8:12], elp[:, 0:4], ALU.add, [gp, elp], [ex])
                        P.tt("dve", ex[:, 4:12], gph[:, :, 1], elp[:, 4:12], ALU.add, [gp, elp], [ex])
                        P.tt("dve", ex[:, 12:16], gp[:, 12:16], elp[:, 12:16], ALU.add, [gp, elp], [ex])
                        P.act(g16[:], ex[:], AF.Exp, [ex], [g16])
                        P.cp("dve", elp[:, 0:4], gp[:, 0:4], [gp], [elp])
                        P.cp("dve", elp[:, 4:12], gph[:, :, 0], [gp], [elp])
                        P.cp("dve", elp[:, 12:16], gp[:, 4:8], [gp], [elp])
                        if scan_stage < 2:
                            continue
                        P.tt("pool", Sm[:], Sm[:], b3(g16[:, 0:4], 4, 514), ALU.mult, [Sm, g16], [Sm])
                        P.cp("act", Smb[:], Sm[:], [Sm], [Smb])
                        P.tt("pool", Sh[:], Sh[:], b3(g16[:, 4:12], 8, 128), ALU.mult, [Sh, g16], [Sh])
                        P.cp("act", Shb[:], Sh[:], [Sh], [Shb])
                        P.tt("pool", Sr[:], Sr[:], b3(g16[:, 12:16], 4, 256), ALU.mult, [Sr, g16], [Sr])
                        P.cp("act", Srb[:], Sr[:], [Sr], [Srb])
                        if scan_stage < 3:
                            continue
                        bh = [outr.nxt(), outr.nxt()]
                        e1, e2 = e1r.nxt(), e2r.nxt()
                        for k in range(2):
                            P.mm(bh[k][0:64, :], CS, lf[:, k * 512:(k + 1) * 512], True, True, [cst, lf], [bh[k]])
                            P.act(e1[:, k * 512:(k + 1) * 512], bh[k][0:64, :], AF.Exp, [bh[k]], [e1])
                            P.act(e2[:, k * 512:(k + 1) * 512], bh[k][0:64, :], AF.Exp, [bh[k]], [e2], scale=-1.0)
                        QA, KA = qar.nxt(), kar.nxt()
                        v3 = lambda ap, h: ap.rearrange("p (h c) -> p h c", h=h)
                        P.tt("dve", QA[:, 1024:2048], hq[:], e1[:], ALU.mult, [hq, e1], [QA])
                        P.tt("pool", KA[:, 1024:2048], kk[:], e2[:], ALU.mult, [kk, e2], [KA])
                        P.tt("dve", v3(QA[:, 0:1024], 4), v3(qk[:, 0:1024], 4), b3(eqs[:, 0:4], 4, 256), ALU.mult,
                             [qk, eqs], [QA])
                        P.tt("pool", v3(KA[:, 0:1024], 4), v3(qk[:, 1024:2048], 4), b3(eks[:, 0:4], 4, 256), ALU.mult,
                             [qk, eks], [KA])
                        P.tt("dve", v3(QA[:, 2048:2560], 4), v3(rq[:, 0:512], 4), b3(eqs[:, 4:8], 4, 128), ALU.mult,
                             [rq, eqs], [QA])
                        P.tt("pool", v3(KA[:, 2048:2560], 4), v3(rq[:, 512:1024], 4), b3(eks[:, 4:8], 4, 128), ALU.mult,
                             [rq, eks], [KA])
                        if scan_stage < 4:
                            continue
                        QT, KT = qtr.nxt(), ktr.nxt()
                        ei_ = 0
                        for (src, dst) in ((QA, QT), (KA, KT)):
                            for g0 in (0, 16):
                                pt = tpr.nxt()
                                n = min(16, 20 - g0)
                                for k in range(n):
                                    u = g0 + k
                                    P.tr(pt[:, k, :], src[:, u * 128:(u + 1) * 128], ident[0:64, 0:64], [src, ident], [pt])
                                P.cp(("act", "dve")[ei_ % 2], dst[:, g0:g0 + n, :], pt[:, 0:n, :], [pt], [dst])
                                ei_ += 1
                        if scan_stage < 5:
                            continue
                        at = [atr.nxt(), atr.nxt()]
                        for hd in range(16):
                            a = at[hd // 8]
                            us = units[hd]
                            for i, u in enumerate(us):
                                P.mm(a[:, hd % 8, :], KT[:, u, :], QT[:, u, :], i == 0, i == len(us) - 1, [KT, QT], [a])
                        pT = ptr_.nxt()
                        for k in range(2):
                            P.op("dve", lambda hh, pT=pT, a=at[k], k=k: hh.copy_predicated(
                                out=pT[:, k * 8:(k + 1) * 8, :], mask=mfull[:].bitcast(mybir.dt.int32), data=a[:]),
                                [at[k], mfull], [pT])
                        if scan_stage < 6:
                            continue
                        Y = yr_.nxt()
                        for h in range(4):
                            po = outr.nxt()
                            P.mm(po[0:64, 0:257], QT[:, 2 * h, :], Smb[:, h, 0:257], True, False, [QT, Smb], [po])
                            P.mm(po[0:64, 0:257], QT[:, 2 * h + 1, :], Smb[:, h, 257:514], False, False, [QT, Smb], [po])
                            P.mm(po[0:64, 0:257], pT[:, h, :], vm[:, h, :], False, True, [pT, vm], [po])
                            dn = dnr.nxt()
                            P.act(dn[:], po[0:64, 256:257], AF.Abs, [po], [dn])
                            P.ts("dve", dn[:], dn[:], 1.0, None, ALU.max, None, [dn], [dn])
                            P.op("dve", lambda hh, dn=dn: hh.reciprocal(out=dn[:], in_=dn[:]), [dn], [dn])
                            P.ts("dve", Y[:, h * 256:(h + 1) * 256], po[0:64, 0:256], dn[:, 0:1], None, ALU.mult, None,
                                 [po, dn], [Y])
                        for k in range(2):
                            po = outr.nxt()
                            for hh in range(4):
                                h = k * 4 + hh
                                cs = slice(hh * 128, (hh + 1) * 128)
                                P.mm(po[0:64, cs], QT[:, 8 + h, :], Shb[:, h, :], True, False, [QT, Shb], [po])
                                P.mm(po[0:64, cs], pT[:, 4 + h, :], hi[:, h * 128:(h + 1) * 128], False, True, [pT, hi], [po])
                            P.cp("act", Y[:, BW + k * 512:BW + (k + 1) * 512], po[0:64, :], [po], [Y])
                        for k in range(2):
                            po = outr.nxt()
                            for hh in range(2):
                                h = k * 2 + hh
                                cs = slice(hh * 256, (hh + 1) * 256)
                                P.mm(po[0:64, cs], QT[:, 16 + h, :], Srb[:, h, :], True, False, [QT, Srb], [po])
                                P.mm(po[0:64, cs], pT[:, 12 + h, :], rv[:, h * 256:(h + 1) * 256], False, True, [pT, rv], [po])
                            P.cp("dve", Y[:, 2 * BW + k * 512:2 * BW + (k + 1) * 512], po[0:64, :], [po], [Y])
                        P.st(Y, S["ysc"][d, r, :], Y[:])
                        if scan_stage < 7:
                            continue
                        for h in range(4):
                            for i in range(2):
                                u = 2 * h + i
                                pd = dsr.nxt()
                                P.mm(pd[:, 0:257], KA[:, u * 128:(u + 1) * 128], vm[:, h, :], True, True, [KA, vm], [pd])
                                P.tt("dve", Sm[:, h, i * 257:(i + 1) * 257], Sm[:, h, i * 257:(i + 1) * 257], pd[:, 0:257],
                                     ALU.add, [Sm, pd], [Sm])
                        for k in range(2):
                            pd = dsr.nxt()
                            for hh in range(4):
                                h = k * 4 + hh
                                P.mm(pd[:, hh * 128:(hh + 1) * 128], KA[:, (8 + h) * 128:(9 + h) * 128],
                                     hi[:, h * 128:(h + 1) * 128], True, True, [KA, hi], [pd])
                            P.tt("dve", Sh[:, k * 4:(k + 1) * 4, :], Sh[:, k * 4:(k + 1) * 4, :], v3(pd[:], 4), ALU.add,
                                 [Sh, pd], [Sh])
                        for k in range(2):
                            pd = dsr.nxt()
                            for hh in range(2):
                                h = k * 2 + hh
                                P.mm(pd[:, hh * 256:(hh + 1) * 256], KA[:, (16 + h) * 128:(17 + h) * 128],
                                     rv[:, h * 256:(h + 1) * 256], True, True, [KA, rv], [pd])
                            P.tt("dve", Sr[:, k * 2:(k + 1) * 2, :], Sr[:, k * 2:(k + 1) * 2, :], v3(pd[:], 2), ALU.add,
                                 [Sr, pd], [Sr])
                    P.end_phase()

            scan_pass(0)
            if stop_after == ("S0", l):
                break
            scan_pass(1)
            if stop_after == ("S1", l):
                break

            with ExitStack() as pes:
                P.begin(pes, "F")
                ident = load_ident(P)
                ng = P.sb("ng", [128, 3 * BW], F32)
                P.ld(ng, ng[:], bcast(I["head_norm_g"][l]))
                yar = P.ring("ya", [128, 3 * BW], F32, 2)
                ybr = P.ring("yb", [128, 3 * BW], F32, 2)
                zr = P.ring("fz", [128, 3 * BW], BF16, 2)
                sqr = P.ring("fsq", [128, 3 * BW], F32, 1)
                gzr = P.ring("fgz", [128, 3 * BW], F32, 1)
                bxr = P.ring("fbx", [128, 3 * BW], BF16, 2)
                btr = P.ring("fbt", [128, 24, 128], BF16, 2)
                mur = P.ring("fmu", [128, 16], F32, 2)
                ssr = P.ring("fss", [128, 16], F32, 2)
                ptr = P.ring("ptf", [128, 8, 128], BF16, 4, psum=True)
                b3 = lambda ap, h, n: ap.unsqueeze(2).to_broadcast([ap.shape[0], h, n])
                v3 = lambda ap, h: ap.rearrange("p (h c) -> p h c", h=h)
                for j in tiles_post:
                    rows = slice(j * 128, (j + 1) * 128)
                    ya, yb, z = yar.nxt(), ybr.nxt(), zr.nxt()
                    P.ld(ya, ya[:], S["ysc"][0, rows, :])
                    P.ld(yb, yb[:], S["ysc"][1, rows, :])
                    P.ld(z, z[:], S["zs"][rows, :])
                    P.tt("dve", ya[:], ya[:], yb[:], ALU.add, [ya, yb], [ya])
                    mu, ss = mur.nxt(), ssr.nxt()
                    for (c0, mc) in ((0, 0), (2 * BW, 4)):
                        y3 = v3(ya[:, c0:c0 + BW], 4)
                        P.op("dve", lambda hh, y3=y3, mu=mu, mc=mc: hh.reduce_sum(out=mu[:, mc:mc + 4], in_=y3,
                                                                               axis=mybir.AxisListType.X), [ya], [mu])
                        P.ts("dve", mu[:, mc:mc + 4], mu[:, mc:mc + 4], -1.0 / 256, None, ALU.mult, None, [mu], [mu])
                        P.tt("dve", y3, y3, b3(mu[:, mc:mc + 4], 4, 256), ALU.add, [ya, mu], [ya])
                    sq = sqr.nxt()
                    P.tt("pool", sq[:], ya[:], ya[:], ALU.mult, [ya], [sq])
                    P.op("dve", lambda hh, sq=sq, ss=ss: hh.reduce_sum(out=ss[:, 0:4], in_=v3(sq[:, 0:BW], 4),
                                                                     axis=mybir.AxisListType.X), [sq], [ss])
                    P.op("dve", lambda hh, sq=sq, ss=ss: hh.reduce_sum(out=ss[:, 4:12], in_=v3(sq[:, BW:2 * BW], 8),
                                                                     axis=mybir.AxisListType.X), [sq], [ss])
                    P.op("dve", lambda hh, sq=sq, ss=ss: hh.reduce_sum(out=ss[:, 12:16], in_=v3(sq[:, 2 * BW:3 * BW], 4),
                                                                     axis=mybir.AxisListType.X), [sq], [ss])
                    P.ts("dve", ss[:, 0:4], ss[:, 0:4], 1.0 / 256, EPS, ALU.mult, ALU.add, [ss], [ss])
                    P.ts("dve", ss[:, 4:12], ss[:, 4:12], 1.0 / 128, EPS, ALU.mult, ALU.add, [ss], [ss])
                    P.ts("dve", ss[:, 12:16], ss[:, 12:16], 1.0 / 256, EPS, ALU.mult, ALU.add, [ss], [ss])
                    P.act(ss[:], ss[:], AF.Sqrt, [ss], [ss])
                    P.op("dve", lambda hh, ss=ss: hh.reciprocal(out=ss[:], in_=ss[:]), [ss], [ss])
                    P.tt("dve", v3(ya[:, 0:BW], 4), v3(ya[:, 0:BW], 4), b3(ss[:, 0:4], 4, 256), ALU.mult, [ya, ss], [ya])
                    P.tt("dve", v3(ya[:, BW:2 * BW], 8), v3(ya[:, BW:2 * BW], 8), b3(ss[:, 4:12], 8, 128), ALU.mult,
                         [ya, ss], [ya])
                    P.tt("dve", v3(ya[:, 2 * BW:3 * BW], 4), v3(ya[:, 2 * BW:3 * BW], 4), b3(ss[:, 12:16], 4, 256),
                         ALU.mult, [ya, ss], [ya])
                    gz = gzr.nxt()
                    P.tt("pool", gz[:], z[:], ng[:], ALU.mult, [z, ng], [gz])
                    bx = bxr.nxt()
                    P.tt("pool", bx[:], ya[:], gz[:], ALU.mult, [ya, gz], [bx])
                    bt = btr.nxt()
                    transpose_to(P, bx, bx, 24, ident, ptr, bt)
                    P.st(bt, S["bxT"][j], bt[:])
                P.end_phase()
            if stop_after == ("F", l):
                break

            with ExitStack() as pes:
                P.begin(pes, "D1")
                ident = load_ident(P)
                wbs = []
                for n in range(3):
                    wb = P.sb("wbr%d" % n, [128, 8, D], BF16)
                    P.ld(wb, wb[:], I["w_branch"][l][n * BW:(n + 1) * BW, :].rearrange("(c p) d -> p c d", p=128),
                         q="pool")
                    wbs.append(wb)
                btr = P.ring("dbt", [128, 24, 128], BF16, 2)
                mgr = P.ring("dmg", [128, 3 * D], BF16, 2)
                mixr = P.ring("dmix", [128, D], F32, 2)
                mixbr = P.ring("dmixb", [128, D], BF16, 2)
                tmpr = P.ring("dtmp", [128, 512], F32, 3)
                mtr = P.ring("dmt", [128, 16, 128], BF16, 2)
                pr = P.ring("pd1", [128, 512], F32, 4, psum=True)
                ptr = P.ring("ptd1", [128, 8, 128], BF16, 2, psum=True)
                for j in tiles_post:
                    rows = slice(j * 128, (j + 1) * 128)
                    bt, mg = btr.nxt(), mgr.nxt()
                    P.ld(bt, bt[:], S["bxT"][j])
                    P.ld(mg, mg[:], S["mg"][rows, :])
                    mixed, mixb = mixr.nxt(), mixbr.nxt()
                    for nb in range(4):
                        nbs = slice(nb * 512, (nb + 1) * 512)
                        for n in range(3):
                            pp = pr.nxt()
                            for c in range(8):
                                P.mm(pp[:], bt[:, n * 8 + c, :], wbs[n][:, c, nbs], c == 0, c == 7, [bt, wbs[n]], [pp])
                            gsl = mg[:, n * D + nb * 512:n * D + (nb + 1) * 512]
                            if n == 0:
                                P.tt("dve", mixed[:, nbs], pp[:], gsl, ALU.mult, [pp, mg], [mixed])
                            else:
                                tmp = tmpr.nxt()
                                P.tt("dve", tmp[:], pp[:], gsl, ALU.mult, [pp, mg], [tmp])
                                dst = mixb if n == 2 else mixed
                                P.tt("pool", dst[:, nbs], mixed[:, nbs], tmp[:], ALU.add, [mixed, tmp], [dst])
                    mt = mtr.nxt()
                    transpose_to(P, mixb, mixb, 16, ident, ptr, mt)
                    P.st(mt, S["mixT"][j], mt[:])
                P.end_phase()
            if stop_after == ("D1", l):
                break

            moe_layer = (l % 2 == 1)
            with ExitStack() as pes:
                P.begin(pes, "D2")
                ident = load_ident(P)
                wo = P.sb("wo", [128, 16, D], BF16)
                P.ld(wo, wo[:], I["w_out"][l].rearrange("(c p) d -> p c d", p=128), q="pool")
                g1 = P.sb("g1", [128, D], F32)
                b1 = P.sb("b1", [128, D], F32)
                P.ld(g1, g1[:], bcast(I["post_ln_g"][l, 0]))
                P.ld(b1, b1[:], bcast(I["post_ln_b"][l, 0]))
                gate1 = P.sb("gate1", [128, D], F32)
                sc2 = P.sb("sc2", [128, D], F32)
                sh2 = P.sb("sh2", [128, D], F32)
                mtr = P.ring("emt", [128, 16, 128], BF16, 2)
                xr = P.ring("ex", [128, D], F32, 2)
                zr = P.ring("ez", [128, D], F32, 2)
                xmr = P.ring("exm", [128, D], F32, 2)
                xnr = P.ring("exn", [128, D], F32, 1)
                ufr = P.ring("euf", [128, D], F32, 1)
                ubr = P.ring("eub", [128, D], BF16, 2)
                utr = P.ring("eut", [128, 16, 128], BF16, 2)
                tmpr = P.ring("etmp", [128, 512], F32, 3)
                str_ = P.ring("est", [128, 4, 6], F32, 2)
                mvr = P.ring("emv", [128, 2], F32, 2)
                rsr = P.ring("ers", [128, 1], F32, 2)
                nmr_ = P.ring("enm", [128, 1], F32, 2)
                pr = P.ring("pd2", [128, 512], F32, 4, psum=True)
                ptr = P.ring("ptd2", [128, 8, 128], BF16, 2, psum=True)
                cur_s = None
                for j in tiles_post:
                    s = 1 if j < 2 else 0
                    if s != cur_s:
                        P.ld(gate1, gate1[:], bcast(S["mod"][l, s, 2 * D:3 * D]))
                        P.ld(sh2, sh2[:], bcast(S["mod"][l, s, 3 * D:4 * D]))
                        P.ld(sc2, sc2[:], bcast(S["mod"][l, s, 4 * D:5 * D]))
                        P.ts("pool", sc2[:], sc2[:], 1.0, 1.0, ALU.add, ALU.mult, [sc2], [sc2])
                        cur_s = s
                    rows = slice(j * 128, (j + 1) * 128)
                    mt, xt, z = mtr.nxt(), xr.nxt(), zr.nxt()
                    P.ld(mt, mt[:], S["mixT"][j])
                    P.ld(xt, xt[:], xsrc(l, j))
                    for nb in range(4):
                        nbs = slice(nb * 512, (nb + 1) * 512)
                        pp = pr.nxt()
                        for c in range(16):
                            P.mm(pp[:], mt[:, c, :], wo[:, c, nbs], c == 0, c == 15, [mt, wo], [pp])
                        tmp = tmpr.nxt()
                        P.tt("dve", tmp[:], pp[:], gate1[:, nbs], ALU.mult, [pp, gate1], [tmp])
                        P.stt("dve", z[:, nbs], xt[:, nbs], ALPHA, tmp[:], ALU.mult, ALU.add, [xt, tmp], [z])
                    st, mv, rstd, nmr = str_.nxt(), mvr.nxt(), rsr.nxt(), nmr_.nxt()
                    ln_stats(P, z, z, st, mv, rstd, nmr)
                    xm = xmr.nxt()
                    P.act(xm[:], z[:], AF.Identity, [z, rstd, nmr], [xm], bias=nmr[:], scale=rstd[:])
                    P.tt("dve", xm[:], xm[:], g1[:], ALU.mult, [xm, g1], [xm])
                    P.tt("pool", xm[:], xm[:], b1[:], ALU.add, [xm, b1], [xm])
                    P.st(xm, S["xmid"][rows, :], xm[:])
                    st, mv, rstd, nmr = str_.nxt(), mvr.nxt(), rsr.nxt(), nmr_.nxt()
                    ln_stats(P, xm, xm, st, mv, rstd, nmr)
                    xn = xnr.nxt()
                    P.act(xn[:], xm[:], AF.Identity, [xm, rstd, nmr], [xn], bias=nmr[:], scale=rstd[:])
                    P.tt("dve", xn[:], xn[:], sc2[:], ALU.mult, [xn, sc2], [xn])
                    ub = ubr.nxt()
                    if moe_layer:
                        uf = ufr.nxt()
                        P.tt("pool", uf[:], xn[:], sh2[:], ALU.add, [xn, sh2], [uf])
                        P.st(uf, S["u2f"][rows, :], uf[:])
                        P.cp("act", ub[:], uf[:], [uf], [ub])
                    else:
                        P.tt("pool", ub[:], xn[:], sh2[:], ALU.add, [xn, sh2], [ub])
                    ut = utr.nxt()
                    transpose_to(P, ub, ub, 16, ident, ptr, ut)
                    P.st(ut, S["u2T"][j], ut[:])
                P.end_phase()
            if stop_after == ("D2", l):
                break

            if moe_layer:
                with ExitStack() as pes:
                    P.begin(pes, "R")
                    wrt = []
                    for e in range(N_EXP):
                        w = P.sb("wr%d" % e, [128, D], F32)
                        P.ld(w, w[:], bcast(I["moe_w_router"][e]))
                        wrt.append(w)
                    ufr = P.ring("ruf", [128, D], F32, 3)
                    jr = P.ring("rj", [128, D], F32, 2)
                    lgr = P.ring("rlg", [128, 8], F32, 2)
                    t8r = P.ring("rt8", [128, 8], F32, 2)
                    m8r = P.ring("rm8", [128, 8], F32, 2)
                    n8r = P.ring("rn8", [128, 8], F32, 2)
                    g8r = P.ring("rg8", [128, 8], F32, 2)
                    s1r = P.ring("rs1", [128, 4], F32, 2)
                    for j in tiles_post:
                        rows = slice(j * 128, (j + 1) * 128)
                        uf = ufr.nxt()
                        P.ld(uf, uf[:], S["u2f"][rows, :])
                        lg = lgr.nxt()
                        for e in range(N_EXP):
                            jk = jr.nxt()
                            P.tt("pool", jk[:], uf[:], wrt[e][:], ALU.mult, [uf, wrt[e]], [jk])
                            P.op("dve", lambda hh, jk=jk, e=e, lg=lg: hh.reduce_sum(
                                out=lg[:, e:e + 1], in_=jk[:], axis=mybir.AxisListType.X), [jk], [lg])
                        s1, t8, m8, n8, g8 = s1r.nxt(), t8r.nxt(), m8r.nxt(), n8r.nxt(), g8r.nxt()
                        AXX = mybir.AxisListType.X
                        P.op("dve", lambda hh, s1=s1, lg=lg: hh.reduce_max(out=s1[:, 0:1], in_=lg[:], axis=AXX), [lg], [s1])
                        P.ts("dve", m8[:], lg[:], s1[:, 0:1], None, ALU.is_equal, None, [lg, s1], [m8])
                        P.stt("dve", t8[:], m8[:], -1e30, lg[:], ALU.mult, ALU.add, [m8, lg], [t8])
                        P.op("dve", lambda hh, s1=s1, t8=t8: hh.reduce_max(out=s1[:, 1:2], in_=t8[:], axis=AXX), [t8], [s1])
                        P.ts("dve", n8[:], t8[:], s1[:, 1:2], None, ALU.is_equal, None, [t8, s1], [n8])
                        P.tt("dve", s1[:, 2:3], s1[:, 1:2], s1[:, 0:1], ALU.subtract, [s1], [s1])
                        P.act(s1[:, 2:3], s1[:, 2:3], AF.Sigmoid, [s1], [s1])
                        P.ts("dve", s1[:, 3:4], s1[:, 2:3], -1.0, 1.0, ALU.mult, ALU.add, [s1], [s1])
                        P.ts("dve", g8[:], m8[:], s1[:, 3:4], None, ALU.mult, None, [m8, s1], [g8])
                        P.stt("dve", g8[:], n8[:], s1[:, 2:3], g8[:], ALU.mult, ALU.add, [n8, s1, g8], [g8])
                        P.st(g8, S["gexp"][rows, :], g8[:])
                    P.end_phase()
            if stop_after == ("R", l):
                break

            with ExitStack() as pes:
                P.begin(pes, "E")
                g2 = P.sb("g2", [128, D], F32)
                b2 = P.sb("b2", [128, D], F32)
                P.ld(g2, g2[:], bcast(I["post_ln_g"][l, 1]))
                P.ld(b2, b2[:], bcast(I["post_ln_b"][l, 1]))
                gate2 = P.sb("gate2", [128, D], F32)
                FG = 2
                if moe_layer:
                    experts = [(I["moe_w_gate"][e], I["moe_w_up"][e], I["moe_w_down"][e], EXP_DIM) for e in range(N_EXP)]
                else:
                    experts = [(I["ffn_w_gate"][l // 2], I["ffn_w_up"][l // 2], I["ffn_w_down"][l // 2], FFN_DIM)]
                wgr = P.ring("fwg", [128, 16, FG * 128], BF16, 2)
                wur = P.ring("fwu", [128, 16, FG * 128], BF16, 2)
                wdr = P.ring("fwd", [128, FG, D], BF16, 2)
                utr = P.ring("fut", [128, 4, 16, 128], BF16, 2)
                hsr = P.ring("fhs", [128, 512], F32, 2)
                hTr = P.ring("fhT", [128, FG, 512], BF16, 2)
                yacc = P.sb("yacc", [128, 4, D], F32)
                gxr = P.ring("fgx", [128, 4, 8], F32, 2)
                xmr = P.ring("fxm", [128, D], F32, 2)
                zr = P.ring("fz2", [128, D], F32, 2)
                str_ = P.ring("fst", [128, 4, 6], F32, 2)
                mvr = P.ring("fmv", [128, 2], F32, 2)
                rsr = P.ring("frs", [128, 1], F32, 2)
                nmr_ = P.ring("fnm", [128, 1], F32, 2)
                pgr = P.ring("pg", [128, 512], F32, 2, psum=True)
                pur = P.ring("pu", [128, 512], F32, 2, psum=True)
                pdr = P.ring("pdn", [128, 512], F32, 4, psum=True)
                supers = []
                if not last:
                    supers.append([0, 1])
                for s0 in range(2, NT, 4):
                    supers.append(list(range(s0, s0 + 4)))
                cur_s = None
                for sup in supers:
                    s = 1 if sup[0] < 2 else 0
                    if s != cur_s:
                        P.ld(gate2, gate2[:], bcast(S["mod"][l, s, 5 * D:6 * D]))
                        cur_s = s
                    nt_ = len(sup)
                    ntok = nt_ * 128
                    ut = utr.nxt()
                    for ti, j in enumerate(sup):
                        P.ld(ut, ut[:, ti, :, :], S["u2T"][j])
                    if moe_layer:
                        gx = gxr.nxt()
                        for ti, j in enumerate(sup):
                            P.ld(gx, gx[:, ti, :], S["gexp"][j * 128:(j + 1) * 128, :])
                    first_acc = True
                    for ei, (Wg, Wu, Wd, FD) in enumerate(experts):
                        for f0 in range(0, FD // 128, FG):
                            wg, wu, wd = wgr.nxt(), wur.nxt(), wdr.nxt()
                            fc = slice(f0 * 128, (f0 + FG) * 128)
                            P.ld(wg, wg[:], Wg[:, fc].rearrange("(c p) f -> p c f", p=128), q="pool")
                            P.ld(wu, wu[:], Wu[:, fc].rearrange("(c p) f -> p c f", p=128), q="pool")
                            P.ld(wd, wd[:], Wd[fc, :].rearrange("(b p) d -> p b d", p=128), q="pool")
                            hT = hTr.nxt()
                            for fb in range(FG):
                                pg, pu = pgr.nxt(), pur.nxt()
                                fs = slice(fb * 128, (fb + 1) * 128)
                                for c in range(16):
                                    P.mm(pg[:, 0:ntok].rearrange("p (t k) -> p t k", k=128), wg[:, c, fs],
                                         ut[:, 0:nt_, c, :], c == 0, c == 15, [wg, ut], [pg])
                                for c in range(16):
                                    P.mm(pu[:, 0:ntok].rearrange("p (t k) -> p t k", k=128), wu[:, c, fs],
                                         ut[:, 0:nt_, c, :], c == 0, c == 15, [wu, ut], [pu])
                                hs = hsr.nxt()
                                P.act(hs[:, 0:ntok], pg[:, 0:ntok], AF.Silu, [pg], [hs])
                                P.tt("dve", hT[:, fb, 0:ntok], hs[:, 0:ntok], pu[:, 0:ntok], ALU.mult, [hs, pu], [hT])
                            for ti in range(nt_):
                                for nb in range(4):
                                    nbs = slice(nb * 512, (nb + 1) * 512)
                                    pd = pdr.nxt()
                                    for fb in range(FG):
                                        P.mm(pd[:], hT[:, fb, ti * 128:(ti + 1) * 128], wd[:, fb, nbs], fb == 0,
                                             fb == FG - 1, [hT, wd], [pd])
                                    if moe_layer:
                                        if first_acc:
                                            P.ts("dve", yacc[:, ti, nbs], pd[:], gx[:, ti, ei:ei + 1], None, ALU.mult, None,
                                                 [pd, gx], [yacc])
                                        else:
                                            P.stt("dve", yacc[:, ti, nbs], pd[:], gx[:, ti, ei:ei + 1], yacc[:, ti, nbs],
                                                  ALU.mult, ALU.add, [pd, gx, yacc], [yacc])
                                    else:
                                        if first_acc:
                                            P.cp("dve", yacc[:, ti, nbs], pd[:], [pd], [yacc])
                                        else:
                                            P.tt("dve", yacc[:, ti, nbs], yacc[:, ti, nbs], pd[:], ALU.add, [yacc, pd], [yacc])
                            first_acc = False
                    for ti, j in enumerate(sup):
                        rows = slice(j * 128, (j + 1) * 128)
                        xm, z = xmr.nxt(), zr.nxt()
                        P.ld(xm, xm[:], S["xmid"][rows, :])
                        P.tt("dve", z[:], yacc[:, ti, :], gate2[:], ALU.mult, [yacc, gate2], [z])
                        P.stt("dve", z[:], xm[:], ALPHA, z[:], ALU.mult, ALU.add, [xm, z], [z])
                        st, mv, rstd, nmr = str_.nxt(), mvr.nxt(), rsr.nxt(), nmr_.nxt()
                        ln_stats(P, z, z, st, mv, rstd, nmr)
                        P.act(xm[:], z[:], AF.Identity, [z, rstd, nmr], [xm], bias=nmr[:], scale=rstd[:])
                        P.tt("dve", xm[:], xm[:], g2[:], ALU.mult, [xm, g2], [xm])
                        P.tt("pool", xm[:], xm[:], b2[:], ALU.add, [xm, b2], [xm])
                        if last:
                            P.st(xm, y_out[(j - 2) * 128:(j - 1) * 128, :], xm[:])
                        else:
                            P.st(xm, S["xcur"][rows, :], xm[:])
                P.end_phase()
            if stop_after == ("E", l):
                break
        P.begin(es)
        fin = P.sb("fin", [128, 8], F32)
        P.memset("pool", fin[:], 0.0, [fin])
        P.end_phase()
    return nc


_CACHE = {}


def prep_inputs(inputs, b, layers=DEPTH):
    f = lambda a: np.ascontiguousarray(np.asarray(a, dtype=np.float32))
    m = {}
    m["x"] = f(inputs["x"][b])
    m["ctx"] = f(inputs["ctx"][b])
    m["c"] = f(np.stack([inputs["c"][b], inputs["c_ctx"]], 0).reshape(2, 16, 128).transpose(2, 1, 0).reshape(128, 32))
    m["w_ada"] = f(inputs["w_ada"])
    m["b_ada"] = f(inputs["b_ada"])
    m["w_in"] = f(inputs["w_in"])
    m["conv_w"] = f(np.asarray(inputs["conv_w"]).reshape(DEPTH, 9, D))
    m["conv_b"] = f(inputs["conv_b"])
    m["mlstm_gate_b"] = f(inputs["mlstm_gate_b"])
    m["hgrn_lb"] = f(inputs["hgrn_lb"])
    m["ret_decay_logit"] = f(np.asarray(inputs["ret_decay_logit"]).reshape(DEPTH, 8))
    m["head_norm_g"] = f(np.asarray(inputs["head_norm_g"]).reshape(DEPTH, 3 * BW))
    m["w_branch"] = f(np.asarray(inputs["w_branch"]).reshape(DEPTH, 3 * BW, D))
    m["w_out"] = f(inputs["w_out"])
    m["post_ln_g"] = f(inputs["post_ln_g"])
    m["post_ln_b"] = f(inputs["post_ln_b"])
    m["ffn_w_gate"] = f(inputs["ffn_w_gate"])
    m["ffn_w_up"] = f(inputs["ffn_w_up"])
    m["ffn_w_down"] = f(inputs["ffn_w_down"])
    if layers > 1:
        m["moe_w_router"] = f(np.asarray(inputs["moe_w_router"])[0].T)
        m["moe_w_gate"] = f(np.asarray(inputs["moe_w_gate"])[0])
        m["moe_w_up"] = f(np.asarray(inputs["moe_w_up"])[0])
        m["moe_w_down"] = f(np.asarray(inputs["moe_w_down"])[0])
    m["consts"] = make_consts()
    return m


def kernel(**inputs):
    if "nc" not in _CACHE:
        _CACHE["nc"] = build_program()
    nc = _CACHE["nc"]
    names = list(build_program.in_names.keys())
    in_maps = [{k: v for k, v in prep_inputs(inputs, b).items() if k in names} for b in range(8)]
    res = run_bass_kernel_spmd(nc, in_maps, core_ids=list(range(8)))
    return np.stack([np.asarray(r["y"], dtype=np.float32) for r in res.results], 0)
```
